# Optimizing a Trainium2 kernel written in Bass

```python
import math
import jax, jax.numpy as jnp
from jax import lax
import numpy as np

D_MODEL = 1024
BATCH = 8
SEQ = 4096
DEPTH = 1

GRID_W = 64
CTX_LEN = 256
MLA_HEADS = 8
MLA_NOPE = 64
MLA_ROPE = 32
MLA_QK = MLA_NOPE + MLA_ROPE
MLA_V = 64
Q_LORA = 256
KV_LORA = 128
MLA_W = MLA_HEADS * MLA_V
MLA_SCALE = MLA_QK ** -0.5
ROPE_THETA = 10000.0
Q_BLOCK = 128
NA_HEADS = 8
NA_DIM = 64
NA_W = NA_HEADS * NA_DIM
NA_WIN_H = 8
NA_WIN_W = 16
NA_SCALE = NA_DIM ** -0.5
D_IN = Q_LORA + KV_LORA + MLA_ROPE + 3 * NA_W + 2 * D_MODEL
N_EXPERTS = 16
D_EXPERT = 1024
EC_CAPACITY = 2
LN_EPS = 1e-5
RMS_EPS = 1e-6
ALPHA = (2.0 * DEPTH) ** 0.25
BETA = (8.0 * DEPTH) ** -0.25

kernel_name = 'hybrid_mla_natten_ec_dit_block'


def _layer_norm(x, g=None, b=None):
    xf = x.astype(jnp.float32)
    mu = jnp.mean(xf, axis=-1, keepdims=True)
    var = jnp.mean(jnp.square(xf - mu), axis=-1, keepdims=True)
    y = (xf - mu) * lax.rsqrt(var + LN_EPS)
    if g is not None:
        y = y * g.astype(jnp.float32) + b.astype(jnp.float32)
    return y.astype(x.dtype)


def _rms_norm(x, g):
    xf = x.astype(jnp.float32)
    y = xf * lax.rsqrt(jnp.mean(jnp.square(xf), axis=-1, keepdims=True) + RMS_EPS)
    return (y * g.astype(jnp.float32)).astype(x.dtype)


def _axial_rope_tables(n):
    t = jnp.arange(n, dtype=jnp.int32)
    row = (t // GRID_W).astype(jnp.float32)
    col = (t % GRID_W).astype(jnp.float32)
    per_axis = MLA_ROPE // 2
    inv_freq = ROPE_THETA ** (-jnp.arange(0, per_axis, 2, dtype=jnp.float32) / per_axis)
    ang = jnp.concatenate([row[:, None] * inv_freq, col[:, None] * inv_freq], axis=-1)
    return jnp.cos(ang), jnp.sin(ang)


def _rope(x, cos, sin):
    half = x.shape[-1] // 2
    x1 = x[..., :half].astype(jnp.float32)
    x2 = x[..., half:].astype(jnp.float32)
    return jnp.concatenate([x1 * cos - x2 * sin, x1 * sin + x2 * cos], axis=-1).astype(x.dtype)


def _modulation(cvec, w_mod, b_mod):
    m = jax.nn.silu(cvec) @ w_mod + b_mod
    return jnp.split(m, 6, axis=-1)


def _split_in(p):
    o1 = Q_LORA
    o2 = o1 + KV_LORA
    o3 = o2 + MLA_ROPE
    o4 = o3 + 3 * NA_W
    return p[..., :o1], p[..., o1:o2], p[..., o2:o3], p[..., o3:o4], p[..., o4:]


def _mla_q(q_c, q_norm_g, w_uq, rope):
    B, n = q_c.shape[:2]
    q = (_rms_norm(q_c, q_norm_g) @ w_uq).reshape(B, n, MLA_HEADS, MLA_QK)
    q_nope, q_rope = q[..., :MLA_NOPE], q[..., MLA_NOPE:]
    if rope is not None:
        cos, sin = rope
        q_rope = _rope(q_rope, cos[:, None], sin[:, None])
    return jnp.concatenate([q_nope, q_rope], axis=-1)


def _mla_kv(kv_c, k_r, kv_norm_g, w_ukv, rope):
    B, n = kv_c.shape[:2]
    kv = (_rms_norm(kv_c, kv_norm_g) @ w_ukv).reshape(B, n, MLA_HEADS, MLA_NOPE + MLA_V)
    k_nope, v = kv[..., :MLA_NOPE], kv[..., MLA_NOPE:]
    if rope is not None:
        cos, sin = rope
        k_r = _rope(k_r, cos, sin)
    k_rope = jnp.broadcast_to(k_r[:, :, None, :], (B, n, MLA_HEADS, MLA_ROPE))
    return jnp.concatenate([k_nope, k_rope], axis=-1), v


def _na_split(na):
    B, n = na.shape[:2]
    qkv = na.reshape(B, n, 3, NA_HEADS, NA_DIM)
    return qkv[:, :, 0], qkv[:, :, 1], qkv[:, :, 2]


def _attend(q, k, v, scale):
    s = jnp.einsum('bqhd,bkhd->bhqk', q, k).astype(jnp.float32) * scale
    p = jax.nn.softmax(s, axis=-1).astype(v.dtype)
    return jnp.einsum('bhqk,bkhd->bqhd', p, v)


def _mla_latent(q, k_all, v_all):
    B, n, H, dq = q.shape
    nb = n // Q_BLOCK
    qb = q.reshape(B, nb, Q_BLOCK, H, dq).transpose(1, 0, 2, 3, 4)
    out = lax.map(lambda qi: _attend(qi, k_all, v_all, MLA_SCALE), qb)
    return out.transpose(1, 0, 2, 3, 4).reshape(B, n, H * MLA_V)


def _natten_latent(q, k, v, k_ctx, v_ctx, rel_bias):
    B, n, H, d = q.shape
    rows = n // GRID_W
    wh = min(NA_WIN_H, rows)
    ww = NA_WIN_W
    qg = q.reshape(B, rows, GRID_W, H, d)
    kg = k.reshape(B, rows, GRID_W, H, d)
    vg = v.reshape(B, rows, GRID_W, H, d)
    col = np.arange(GRID_W)
    col_start = np.clip(col - ww // 2, 0, GRID_W - ww)
    col_idx = col_start[:, None] + np.arange(ww)[None, :]
    dc = col_idx - col[:, None] + (NA_WIN_W - 1)
    nk = wh * ww

    def row_fn(r):
        r0 = jnp.clip(r - wh // 2, 0, rows - wh)
        q_r = lax.dynamic_index_in_dim(qg, r, axis=1, keepdims=False)
        k_band = lax.dynamic_slice_in_dim(kg, r0, wh, axis=1)
        v_band = lax.dynamic_slice_in_dim(vg, r0, wh, axis=1)
        k_win = k_band[:, :, col_idx].transpose(0, 2, 1, 3, 4, 5).reshape(B, GRID_W, nk, H, d)
        v_win = v_band[:, :, col_idx].transpose(0, 2, 1, 3, 4, 5).reshape(B, GRID_W, nk, H, d)
        dr = r0 + jnp.arange(wh) - r + (NA_WIN_H - 1)
        bias = rel_bias[:, dr[:, None, None], dc[None]]
        bias = bias.transpose(0, 2, 1, 3).reshape(H, GRID_W, nk)
        s_loc = jnp.einsum('bqhd,bqkhd->bhqk', q_r, k_win).astype(jnp.float32) * NA_SCALE + bias[None].astype(jnp.float32)
        s_ctx = jnp.einsum('bqhd,bkhd->bhqk', q_r, k_ctx).astype(jnp.float32) * NA_SCALE
        p = jax.nn.softmax(jnp.concatenate([s_loc, s_ctx], axis=-1), axis=-1).astype(v.dtype)
        return (jnp.einsum('bhqk,bqkhd->bqhd', p[..., :nk], v_win)
                + jnp.einsum('bhqk,bkhd->bqhd', p[..., nk:], v_ctx))

    out = lax.map(row_fn, jnp.arange(rows, dtype=jnp.int32))
    return out.transpose(1, 0, 2, 3, 4).reshape(B, n, H * d)


def _merge(y_mla, y_na, gates, w_proj_mla, w_proj_na, w_out):
    g_mla, g_na = jnp.split(gates, 2, axis=-1)
    return (jax.nn.sigmoid(g_mla) * (y_mla @ w_proj_mla) + jax.nn.sigmoid(g_na) * (y_na @ w_proj_na)) @ w_out


def _expert_choice_ffn(u, w_router, w_exp_gate, w_exp_up, w_exp_down):
    B, n, D = u.shape
    cap = EC_CAPACITY * n // N_EXPERTS
    aff = jax.nn.softmax((u @ w_router).astype(jnp.float32), axis=-1)
    g, idx = lax.top_k(aff.transpose(0, 2, 1), cap)
    xe = jax.vmap(lambda ub, ib: ub[ib])(u, idx)
    h = jax.nn.silu(jnp.einsum('becd,edf->becf', xe, w_exp_gate)) * jnp.einsum('becd,edf->becf', xe, w_exp_up)
    ye = jnp.einsum('becf,efd->becd', h, w_exp_down) * g[..., None].astype(u.dtype)
    return jax.vmap(lambda yb, ib: jnp.zeros((n, D), yb.dtype).at[ib.reshape(-1)].add(yb.reshape(-1, D)))(ye, idx)


def _layer(x, ctx, c, c_ctx, lp, rope, update_ctx):
    B, n, _ = x.shape
    L = ctx.shape[1]
    sh1, sc1, g1, sh2, sc2, g2 = [m[:, None, :] for m in _modulation(c, lp['w_mod'], lp['b_mod'])]
    csh1, csc1, cg1, csh2, csc2, cg2 = _modulation(c_ctx, lp['w_mod'], lp['b_mod'])

    u = _layer_norm(x) * (1 + sc1) + sh1
    uc = _layer_norm(ctx) * (1 + csc1) + csh1
    q_c, kv_c, k_r, na, gates = _split_in(u @ lp['w_in'])
    cq_c, ckv_c, ck_r, cna, cgates = _split_in(uc @ lp['w_in'])

    mq = _mla_q(q_c, lp['q_norm_g'], lp['w_uq'], rope)
    mk, mv = _mla_kv(kv_c, k_r, lp['kv_norm_g'], lp['w_ukv'], rope)
    cmk, cmv = _mla_kv(ckv_c, ck_r, lp['kv_norm_g'], lp['w_ukv'], None)
    y_mla = _mla_latent(mq, jnp.concatenate([cmk, mk], axis=1), jnp.concatenate([cmv, mv], axis=1))

    nq, nk, nv = _na_split(na)
    cnq, cnk, cnv = _na_split(cna)
    y_na = _natten_latent(nq, nk, nv, cnk, cnv, lp['na_rel_bias'])

    mix = _merge(y_mla, y_na, gates, lp['w_proj_mla'], lp['w_proj_na'], lp['w_out'])
    x_new = _layer_norm(ALPHA * x + g1 * mix, lp['ln1_g'], lp['ln1_b'])

    u2 = _layer_norm(x_new) * (1 + sc2) + sh2
    moe = _expert_choice_ffn(u2, lp['w_router'], lp['w_exp_gate'], lp['w_exp_up'], lp['w_exp_down'])
    x_new = _layer_norm(ALPHA * x_new + g2 * moe, lp['ln2_g'], lp['ln2_b'])

    if update_ctx:
        cmq = _mla_q(cq_c, lp['q_norm_g'], lp['w_uq'], None)
        yc_mla = _attend(cmq, cmk, cmv, MLA_SCALE).reshape(B, L, MLA_W)
        yc_na = _attend(cnq, cnk, cnv, NA_SCALE).reshape(B, L, NA_W)
        cmix = _merge(yc_mla, yc_na, cgates, lp['w_proj_mla'], lp['w_proj_na'], lp['w_out'])
        ctx = _layer_norm(ALPHA * ctx + cg1 * cmix, lp['ln1_g'], lp['ln1_b'])
        uc2 = _layer_norm(ctx) * (1 + csc2) + csh2
        cmoe = _expert_choice_ffn(uc2, lp['w_router'], lp['w_exp_gate'], lp['w_exp_up'], lp['w_exp_down'])
        ctx = _layer_norm(ALPHA * ctx + cg2 * cmoe, lp['ln2_g'], lp['ln2_b'])
    return x_new, ctx


def setup_inputs(seed: int = 0) -> dict:
    key = jax.random.key(seed)
    ks = jax.random.split(key, 26)

    def nrm(k, shape, scale):
        return jax.random.normal(k, shape, jnp.float32) * scale

    Ld = DEPTH
    return {
        'x': nrm(ks[0], (BATCH, SEQ, D_MODEL), 1.0),
        'c': nrm(ks[1], (BATCH, D_MODEL), 1.0),
        'ctx': nrm(ks[2], (BATCH, CTX_LEN, D_MODEL), 1.0),
        'c_ctx': nrm(ks[3], (D_MODEL,), 1.0),
        'w_mod': nrm(ks[4], (Ld, D_MODEL, 6 * D_MODEL), 0.5 * D_MODEL ** -0.5),
        'b_mod': nrm(ks[5], (Ld, 6 * D_MODEL), 0.02),
        'w_in': nrm(ks[6], (Ld, D_MODEL, D_IN), D_MODEL ** -0.5),
        'q_norm_g': 1.0 + nrm(ks[7], (Ld, Q_LORA), 0.02),
        'w_uq': nrm(ks[8], (Ld, Q_LORA, MLA_HEADS * MLA_QK), Q_LORA ** -0.5),
        'kv_norm_g': 1.0 + nrm(ks[9], (Ld, KV_LORA), 0.02),
        'w_ukv': nrm(ks[10], (Ld, KV_LORA, MLA_HEADS * (MLA_NOPE + MLA_V)), KV_LORA ** -0.5),
        'na_rel_bias': nrm(ks[11], (Ld, NA_HEADS, 2 * NA_WIN_H - 1, 2 * NA_WIN_W - 1), 0.1),
        'w_proj_mla': nrm(ks[12], (Ld, MLA_W, D_MODEL), MLA_W ** -0.5),
        'w_proj_na': nrm(ks[13], (Ld, NA_W, D_MODEL), NA_W ** -0.5),
        'w_out': nrm(ks[14], (Ld, D_MODEL, D_MODEL), BETA * D_MODEL ** -0.5),
        'ln1_g': 1.0 + nrm(ks[15], (Ld, D_MODEL), 0.02),
        'ln1_b': nrm(ks[16], (Ld, D_MODEL), 0.02),
        'w_router': nrm(ks[17], (Ld, D_MODEL, N_EXPERTS), D_MODEL ** -0.5),
        'w_exp_gate': nrm(ks[18], (Ld, N_EXPERTS, D_MODEL, D_EXPERT), D_MODEL ** -0.5),
        'w_exp_up': nrm(ks[19], (Ld, N_EXPERTS, D_MODEL, D_EXPERT), D_MODEL ** -0.5),
        'w_exp_down': nrm(ks[20], (Ld, N_EXPERTS, D_EXPERT, D_MODEL), BETA * D_EXPERT ** -0.5),
        'ln2_g': 1.0 + nrm(ks[21], (Ld, D_MODEL), 0.02),
        'ln2_b': nrm(ks[22], (Ld, D_MODEL), 0.02),
    }


def reference(x, c, ctx, c_ctx, w_mod, b_mod, w_in, q_norm_g, w_uq, kv_norm_g, w_ukv, na_rel_bias,
              w_proj_mla, w_proj_na, w_out, ln1_g, ln1_b, w_router, w_exp_gate, w_exp_up, w_exp_down,
              ln2_g, ln2_b):
    n = x.shape[1]
    rope = _axial_rope_tables(n)
    for l in range(DEPTH):
        lp = dict(w_mod=w_mod[l], b_mod=b_mod[l], w_in=w_in[l], q_norm_g=q_norm_g[l], w_uq=w_uq[l],
                  kv_norm_g=kv_norm_g[l], w_ukv=w_ukv[l], na_rel_bias=na_rel_bias[l],
                  w_proj_mla=w_proj_mla[l], w_proj_na=w_proj_na[l], w_out=w_out[l],
                  ln1_g=ln1_g[l], ln1_b=ln1_b[l], w_router=w_router[l], w_exp_gate=w_exp_gate[l],
                  w_exp_up=w_exp_up[l], w_exp_down=w_exp_down[l], ln2_g=ln2_g[l], ln2_b=ln2_b[l])
        x, ctx = _layer(x, ctx, c, c_ctx, lp, rope, update_ctx=(l < DEPTH - 1))
    return x
```

```python
import numpy as np
from contextlib import ExitStack
import concourse.bass as bass
import concourse.mybir as mybir
from concourse.bass_utils import run_bass_kernel_spmd

F32 = mybir.dt.float32
BF = mybir.dt.bfloat16
I32 = mybir.dt.int32
ALU = mybir.AluOpType
AF = mybir.ActivationFunctionType
AX = mybir.AxisListType

D = 1024
SEQ = 4096
CTX = 256
NKEY = SEQ + CTX
NH = 8
LN_EPS = 1e-5
RMS_EPS = 1e-6
ALPHA = 2.0 ** 0.25
MLA_SCALE = 96.0 ** -0.5
NE = 16
CAP = 512
NEG = -30000.0
KD = 8
NBIS = 30


class Res:
    __slots__ = ("lw", "rd", "name", "psum")

    def __init__(self, name="", psum=False):
        self.lw = None
        self.rd = {}
        self.name = name
        self.psum = psum


class Prog:
    def __init__(self, nc, es):
        self.nc = nc
        self.sems = []
        self.ops = {e: [] for e in ("pe", "act", "dve", "pool", "sp")}
        self.esem = {}
        for e in ("pe", "act", "dve", "pool"):
            self.esem[e] = self._newsem(es, "s_" + e)
        self.cnt = {e: 0 for e in self.esem}
        self.waited = {e: {} for e in self.ops}
        self.dsem = {q: [self._newsem(es, f"d_{q}{i}") for i in range(KD)] for q in ("sp", "pool")}
        self.dcnt = {q: [0] * KD for q in ("sp", "pool")}
        self.dn = {"sp": 0, "pool": 0}

    def _newsem(self, es, name):
        s = es.enter_context(self.nc.semaphore(name))
        self.sems.append(s)
        return len(self.sems) - 1

    def _deps(self, eng, reads, writes, extra=(), is_dma=False):
        deps = {}

        def add(tok):
            if tok is None:
                return
            s, v = tok
            if deps.get(s, 0) < v:
                deps[s] = v
        own = self.esem.get(eng) if not is_dma else None
        for r in reads:
            add(r.lw)
            if r.psum:
                for s, v in r.rd.items():
                    if s != self.esem.get(eng):
                        add((s, v))
        for w in writes:
            if w.lw is not None and w.lw[0] != own:
                add(w.lw)
            for s, v in w.rd.items():
                if s != own:
                    add((s, v))
        for t in extra:
            add(t)
        waits = []
        for s, v in deps.items():
            if eng == "pe" and s == self.esem["pe"]:
                continue
            if self.waited[eng].get(s, 0) < v:
                self.waited[eng][s] = v
                waits.append((s, v))
        return waits

    def _mark(self, tok, reads, writes):
        s, v = tok
        for w in writes:
            w.lw = tok
            w.rd = {}
        for r in reads:
            if r.rd.get(s, 0) < v:
                r.rd[s] = v

    @staticmethod
    def _flat(xs):
        out = []
        for x in xs:
            if isinstance(x, (list, tuple)):
                out.extend(Prog._flat(x))
            else:
                out.append(x)
        return out

    def op(self, eng, fn, reads=(), writes=()):
        reads = self._flat(reads); writes = self._flat(writes)
        waits = self._deps(eng, reads, writes)
        self.cnt[eng] += 1
        tok = (self.esem[eng], self.cnt[eng])
        self.ops[eng].append((waits, fn, self.esem[eng], 1))
        self._mark(tok, reads, writes)
        return tok

    def dma(self, q, fn, reads=(), writes=()):
        reads = self._flat(reads); writes = self._flat(writes)
        i = self.dn[q] % KD
        self.dn[q] += 1
        s = self.dsem[q][i]
        prev = (s, 16 * self.dcnt[q][i]) if self.dcnt[q][i] > 0 else None
        waits = self._deps(q, reads, writes, extra=(prev,), is_dma=True)
        self.dcnt[q][i] += 1
        tok = (s, 16 * self.dcnt[q][i])
        self.ops[q].append((waits, fn, s, 16))
        self._mark(tok, reads, writes)
        return tok

    def barrier(self):
        targets = []
        for e, s in self.esem.items():
            if self.cnt[e] > 0:
                targets.append((s, self.cnt[e]))
        for q in ("sp", "pool"):
            for i in range(KD):
                if self.dcnt[q][i] > 0:
                    targets.append((self.dsem[q][i], 16 * self.dcnt[q][i]))
        for eng in self.ops:
            waits = []
            for s, v in targets:
                if eng == "pe" and s == self.esem["pe"]:
                    continue
                if self.waited[eng].get(s, 0) < v:
                    self.waited[eng][s] = v
                    waits.append((s, v))
            if waits:
                self.ops[eng].append((waits, None, None, 0))

    def emit(self, block):
        sems = self.sems

        def run(eng_name):
            def body(e):
                for waits, fn, s, inc in self.ops[eng_name]:
                    for ws, wv in waits:
                        e.wait_ge(sems[ws], wv)
                    if fn is not None:
                        fn(e).then_inc(sems[s], inc)
            return body
        block.tensor(run("pe"))
        block.scalar(run("act"))
        block.vector(run("dve"))
        block.gpsimd(run("pool"))
        block.sync(run("sp"))


def build_program(debug=False):
    nc = bass.Bass("TRN2", target_bir_lowering=False)

    def din(name, shape, dt=F32):
        return nc.dram_tensor(name, list(shape), dt, kind="ExternalInput").ap()
    x_d = din("x", [SEQ, D])
    ctx_d = din("ctx", [CTX, D])
    cc_d = din("cc", [128, 8, 2])
    wmod_d = din("w_mod", [D, 6 * D])
    bmodfm_d = din("bmod_fm", [128, 48])
    bmodrow_d = din("bmod_row", [1, 6 * D])
    win_d = din("w_in", [D, 4000])
    qg_d = din("qg", [128, 2])
    kvg_d = din("kvg", [128, 1])
    wuq_d = din("w_uq", [256, 768])
    wukv_d = din("w_ukv", [128, 1024])
    natab_d = din("natab", [10, NH, 5, 64, 128])
    wpm_d = din("w_proj_mla", [512, D])
    wpn_d = din("w_proj_na", [512, D])
    wout_d = din("w_out", [D, D])
    lnv_d = din("lnv", [4, D])
    wr_d = din("w_router", [D, NE])
    wg_d = din("w_exp_gate", [NE, D, D])
    wu_d = din("w_exp_up", [NE, D, D])
    wd_d = din("w_exp_down", [NE, D, D])
    ropec_d = din("ropec", [96, SEQ])
    ropes_d = din("ropes", [96, SEQ])
    out_d = nc.dram_tensor("out", [SEQ, D], F32, kind="ExternalOutput").ap()
    skind = "ExternalOutput" if debug else "Internal"
    u2_d = nc.dram_tensor("u2_d", [SEQ, D], BF, kind=skind).ap()
    acc_d = nc.dram_tensor("acc_d", [SEQ, D], F32, kind=skind).ap()
    yna_d = nc.dram_tensor("yna_d", [512, SEQ], BF, kind=skind).ap()
    g2_d = nc.dram_tensor("g2_d", [128, D], F32, kind="Internal").ap()

    with ExitStack() as es:
        E = es.enter_context
        AW = 52900
        arena = E(nc.sbuf_tensor("arena", [128, AW], F32))
        banks = [E(nc.psum_tensor(f"bank{i}", [128, 512], F32)) for i in range(8)]
        bR = [Res(f"bank{i}", psum=True) for i in range(8)]
        P = Prog(nc, es)
        block = E(nc.Block())

        cur = [0]
        lim = [AW]

        def alloc(shape, dt=F32):
            n = int(np.prod(shape[1:]))
            words = (n * (4 if dt in (F32, I32) else 2) + 3) // 4
            words = (words + 7) // 8 * 8
            off = cur[0]
            cur[0] += words
            assert cur[0] <= lim[0], f"arena overflow {cur[0]} > {lim[0]}"
            v = arena[:, off:off + words]
            if dt != F32:
                v = v.bitcast(dt)
            v = v[:, 0:n]
            if len(shape) == 3:
                v = v.rearrange("p (a b) -> p a b", b=shape[2])
            elif len(shape) == 4:
                v = v.rearrange("p (a b c) -> p a b c", b=shape[2], c=shape[3])
            elif len(shape) == 5:
                v = v.rearrange("p (a b c d) -> p a b c d", b=shape[2], c=shape[3], d=shape[4])
            return v

        def bcast_mid(ap2, n):
            a = ap2.ap
            return bass.AP(ap2.tensor, ap2.offset, [list(a[0]), [0, n], list(a[1])])

        def bcast_part(dap_row, n):
            a = dap_row.ap
            return bass.AP(dap_row.tensor, dap_row.offset, [[0, 128], list(a[-1])])

        def mm(out, lhsT, rhs, start=True, stop=True, R=(), W=()):
            return P.op("pe", lambda e: e.matmul(out, lhsT=lhsT, rhs=rhs, start=start, stop=stop), R, W)

        def tr(out, in_, ident, R=(), W=()):
            return P.op("pe", lambda e: e.transpose(out=out, in_=in_, identity=ident), R, W)

        def act(out, in_, func, R=(), W=(), **kw):
            return P.op("act", lambda e: e.activation(out=out, in_=in_, func=func, **kw), R, W)

        def tt(eng, out, in0, in1, op, R=(), W=()):
            return P.op(eng, lambda e: e.tensor_tensor(out=out, in0=in0, in1=in1, op=op), R, W)

        def ts(eng, out, in0, s1, s2, op0, op1=None, R=(), W=()):
            if op1 is None:
                return P.op(eng, lambda e: e.tensor_scalar(out=out, in0=in0, scalar1=s1, scalar2=None, op0=op0), R, W)
            return P.op(eng, lambda e: e.tensor_scalar(out=out, in0=in0, scalar1=s1, scalar2=s2, op0=op0, op1=op1), R, W)

        def stt(eng, out, in0, scalar, in1, op0, op1, R=(), W=()):
            return P.op(eng, lambda e: e.scalar_tensor_tensor(out=out, in0=in0, scalar=scalar, in1=in1, op0=op0, op1=op1), R, W)

        def cp(eng, out, in_, R=(), W=()):
            if eng == "act":
                return P.op("act", lambda e: e.activation(out=out, in_=in_, func=AF.Copy), R, W)
            return P.op(eng, lambda e: e.tensor_copy(out=out, in_=in_), R, W)

        def recip(out, in_, R=(), W=()):
            return P.op("dve", lambda e: e.reciprocal(out=out, in_=in_), R, W)

        def memset(eng, ap, val, W=()):
            return P.op(eng, lambda e: e.memset(ap, val), (), W)

        def dma(q, out, in_, R=(), W=()):
            return P.dma(q, lambda e: e.dma_start(out=out, in_=in_), R, W)

        ident_bf = alloc([128, 128], BF); r_ident = Res()
        ident_f = alloc([128, 128], F32)
        ones_bf = alloc([128, 128], BF)
        ones_f = alloc([128, 128], F32)
        tri_f = alloc([128, 128], F32)
        neghalf = alloc([128, 8], F32)
        r_const = Res("const")
        modfm = alloc([128, 16, 2], F32); r_modfm = Res()
        sc2p1_bc = alloc([128, D]); sh2_bc = alloc([128, D])
        ln1g_bc = alloc([128, D]); ln1b_bc = alloc([128, D])
        r_bc = Res("bc")
        aff = alloc([128, 32, NE]); r_aff = Res("aff")
        g1_bc = alloc([128, D]); r_g1 = Res()
        st6 = [alloc([128, 12]) for _ in range(10)]
        mv = [alloc([128, 2]) for _ in range(10)]
        rstd = [alloc([128, 1]) for _ in range(10)]
        nbias = [alloc([128, 1]) for _ in range(10)]
        r_ln = [Res(f"ln{i}") for i in range(10)]
        PERSIST = cur[0]

        memset("pool", ident_bf[:, :], 1.0, W=[r_const])
        P.op("pool", lambda e: e.affine_select(out=ident_bf[:, :], in_=ident_bf[:, :], pattern=[[-1, 128]], compare_op=ALU.is_equal,
                                               fill=0.0, base=0, channel_multiplier=1), [r_const], [r_const])
        memset("pool", ident_f[:, :], 1.0, W=[r_const])
        P.op("pool", lambda e: e.affine_select(out=ident_f[:, :], in_=ident_f[:, :], pattern=[[-1, 128]], compare_op=ALU.is_equal,
                                               fill=0.0, base=0, channel_multiplier=1), [r_const], [r_const])
        memset("pool", ones_bf[:, :], 1.0, W=[r_const])
        memset("pool", ones_f[:, :], 1.0, W=[r_const])
        memset("pool", tri_f[:, :], 1.0, W=[r_const])
        P.op("pool", lambda e: e.affine_select(out=tri_f[:, :], in_=tri_f[:, :], pattern=[[1, 128]], compare_op=ALU.is_gt,
                                               fill=0.0, base=0, channel_multiplier=-1), [r_const], [r_const])
        memset("pool", neghalf[:, :], -0.5, W=[r_const])

        cur[0] = PERSIST
        cc_sb = alloc([128, 8, 2]); scc = alloc([128, 8, 2]); r_cc = Res()
        bmodfm = alloc([128, 48]); r_bm = Res()
        wm1 = alloc([128, 8, 2048]); r_wm1 = Res()
        r_g2d = Res()
        dma("sp", cc_sb[:, :, :], cc_d[:, :, :], W=[r_cc])
        dma("sp", bmodfm[:, :], bmodfm_d[:, :], W=[r_bm])
        dma("sp", wm1[:, :, :], wmod_d[:, 0:2048].rearrange("(k p) n -> p k n", p=128), W=[r_wm1])
        for i, t in enumerate((ln1g_bc, ln1b_bc)):
            dma("sp", t[:, :], bcast_part(lnv_d[i:i + 1, :], 128), W=[r_bc])
        act(scc[:, :, :], cc_sb[:, :, :], AF.Silu, R=[r_cc], W=[r_cc])
        pm = banks[0]
        for fc in range(16):
            for k in range(8):
                mm(pm[:, fc * 2:fc * 2 + 2], wm1[:, k, fc * 128:(fc + 1) * 128], scc[:, k, :], start=(k == 0), stop=(k == 7),
                   R=[r_wm1, r_cc], W=[bR[0]])
        pm3 = pm[:, 0:32].rearrange("p (a b) -> p a b", b=2)
        for j in range(2):
            tt("dve", modfm[:, :, j], pm3[:, :, j], bmodfm[:, 0:16], ALU.add, R=[bR[0], r_bm], W=[r_modfm])
        ts("dve", modfm[:, 8:16, :], modfm[:, 8:16, :], 1.0, None, ALU.add, R=[r_modfm], W=[r_modfm])
        def ln_stats(src, rsrc, i, eps=LN_EPS):
            rl = r_ln[i]
            P.op("dve", lambda e: e.bn_stats(out=st6[i][:, 0:6], in_=src[:, 0:512]), [rsrc], [rl])
            P.op("dve", lambda e: e.bn_stats(out=st6[i][:, 6:12], in_=src[:, 512:1024]), [rsrc], [rl])
            P.op("dve", lambda e: e.bn_aggr(out=mv[i][:, :], in_=st6[i][:, :]), [rl], [rl])
            ts("dve", rstd[i][:, :], mv[i][:, 1:2], eps, None, ALU.add, R=[rl], W=[rl])
            tt("pool", rstd[i][:, :], rstd[i][:, :], neghalf[:, 0:1], ALU.pow, R=[rl, r_const], W=[rl])
            stt("dve", nbias[i][:, :], mv[i][:, 0:1], -1.0, rstd[i][:, :], ALU.mult, ALU.mult, R=[rl], W=[rl])

        UTMP = [None, None]

        def run_il(gens):
            gens = list(gens)
            while gens:
                for g in list(gens):
                    try:
                        next(g)
                    except StopIteration:
                        gens.remove(g)

        def run_stag(items):
            items = [[st, g] for st, g in items]
            tick = 0
            while items:
                for it in list(items):
                    if it[0] > tick:
                        continue
                    try:
                        next(it[1])
                    except StopIteration:
                        items.remove(it)
                tick += 1

        def ln_stats_g(src, rsrc, i, eps=LN_EPS):
            rl = r_ln[i]
            P.op("dve", lambda e: e.bn_stats(out=st6[i][:, 0:6], in_=src[:, 0:512]), [rsrc], [rl])
            P.op("dve", lambda e: e.bn_stats(out=st6[i][:, 6:12], in_=src[:, 512:1024]), [rsrc], [rl])
            yield
            P.op("dve", lambda e: e.bn_aggr(out=mv[i][:, :], in_=st6[i][:, :]), [rl], [rl])
            ts("dve", rstd[i][:, :], mv[i][:, 1:2], eps, None, ALU.add, R=[rl], W=[rl])
            yield
            tt("pool", rstd[i][:, :], rstd[i][:, :], neghalf[:, 0:1], ALU.pow, R=[rl, r_const], W=[rl])
            yield
            stt("dve", nbias[i][:, :], mv[i][:, 0:1], -1.0, rstd[i][:, :], ALU.mult, ALU.mult, R=[rl], W=[rl])

        def ln_stats(src, rsrc, i, eps=LN_EPS):
            for _ in ln_stats_g(src, rsrc, i, eps):
                pass

        def ln0_tile(src_d, row0, t, mcol, xts, r_xts, xh, r_xh, uT, r_uT, load, lnset, trbank, skew=0):
            xi = t % 2
            i = lnset
            ptb = banks[trbank].bitcast(BF).rearrange("p (a b) -> p a b", b=128)
            if load:
                dma("sp", xts[xi][:, :], src_d[row0 + t * 128:row0 + (t + 1) * 128, :], W=[r_xts[xi]])
            yield
            yield from ln_stats_g(xts[xi], r_xts[xi], i)
            act(xh[xi][:, :], xts[xi][:, :], AF.Identity, R=[r_xts[xi], r_ln[i]], W=[r_xh[xi]],
                scale=rstd[i][:, 0:1], bias=nbias[i][:, 0:1])
            yield
            for _ in range(skew):
                yield
            for fc in range(8):
                tr(ptb[:, fc, :], xh[xi][:, fc * 128:(fc + 1) * 128], ident_bf[:, :], R=[r_xh[xi], r_const], W=[bR[trbank]])
            pst = modfm.ap[0][0]
            sc_b = bass.AP(modfm.tensor, modfm.offset + 16 + mcol, [[pst, 128], [2, 8], [0, 128]])
            sh_b = bass.AP(modfm.tensor, modfm.offset + mcol, [[pst, 128], [2, 8], [0, 128]])
            tmpu, r_tmpu = UTMP[0], UTMP[1]
            tt("dve", tmpu[:, :, :], ptb[:, :, :], sc_b, ALU.mult, R=[bR[trbank], r_modfm], W=[r_tmpu])
            tt("pool", uT[:, :, t * 128:(t + 1) * 128], tmpu[:, :, :], sh_b, ALU.add, R=[r_tmpu, r_modfm], W=[r_uT[t]])
            yield

        def ln0_block(src_d, row0, ntiles, mcol, xts, r_xts, xh, r_xh, uT, r_uT, keep=False, preloaded=0, trbanks=(0, 0)):
            for t0 in range(0, ntiles, 2):
                run_il([ln0_tile(src_d, row0, t, mcol, xts, r_xts, xh, r_xh, uT, r_uT, t >= preloaded, 4 + t % 2, trbanks[t % 2])
                        for t in range(t0, min(ntiles, t0 + 2))])

        P.barrier()
        cur[0] = PERSIST
        qnT = alloc([128, 2, SEQ], BF); r_qn = [Res() for _ in range(8)]
        kvnT = alloc([128, NKEY], BF); r_kvn = Res()
        krT = alloc([128, NKEY], BF); r_kr = Res()
        LAT_END = cur[0]
        wna = alloc([128, 8, 1536], BF); r_wna = Res()
        wq = alloc([128, 8, 256], BF); wkv = alloc([128, 8, 128], BF)
        wkr = alloc([128, 8, 96], BF); wkrr = alloc([128, 8, 96], BF); r_wl = Res()
        sqb = alloc([128, 2, 512], BF); r_sqb = Res()
        rr_t = alloc([128, 512]); r_rr = Res()
        cqb = alloc([128, 512]); sqb_t = alloc([128, 512]); r_rope = Res()
        tb01 = alloc([128, NH, 9, 128], BF); r_tb01 = Res()
        tbe = alloc([128, NH, 4, 128], BF); r_tbe = Res()
        xts = [alloc([128, D]) for _ in range(2)]; r_xts = [Res(), Res()]
        xh = [alloc([128, D], BF) for _ in range(2)]; r_xh = [Res(), Res()]
        uT = alloc([128, 8, 512], BF); r_uT = [Res() for _ in range(4)]
        UTMP[0] = alloc([128, 8, 128]); UTMP[1] = Res()
        utflat = UTMP[0].rearrange("p a b -> p (a b)")
        t1 = utflat[:, 0:512]; t2 = utflat[:, 512:1024]; r_t1 = UTMP[1]; r_t2 = UTMP[1]
        nqT = [alloc([128, 4, 512], BF) for _ in range(2)]; r_nq = [Res(), Res()]
        NSLOT = 12
        RING = 10
        nkT = alloc([128, 4, NSLOT * 128], BF); r_nk = [Res() for _ in range(NSLOT)]
        nva = alloc([128, NSLOT, NH, 128], BF); r_nv = [Res() for _ in range(NSLOT)]
        pT = [alloc([128, 448], BF) for _ in range(4)]; r_pT = [Res() for _ in range(4)]
        rcn = [alloc([128, 64]) for _ in range(4)]; r_rcn = [Res() for _ in range(4)]
        yblk = [alloc([128, 4, 512], BF) for _ in range(2)]; r_yblk = [Res(), Res()]
        r_yna = [Res() for _ in range(8)]

        dma("pool", wna[:, :, :], win_d[:, 416:1952].rearrange("(k p) n -> p k n", p=128), W=[r_wna])
        memset("pool", nva[:, :, :, 64:128], 1.0, W=r_nv)

        w3 = win_d.rearrange("(k p) n -> p k n", p=128)
        memset("dve", wkr[:, :, 0:64], 0.0, W=[r_wl])
        memset("dve", wkrr[:, :, 0:64], 0.0, W=[r_wl])
        dma("pool", wq[:, :, :], w3[:, :, 0:256], W=[r_wl])
        dma("pool", wkv[:, :, :], w3[:, :, 256:384], W=[r_wl])
        dma("pool", wkr[:, :, 64:96], w3[:, :, 384:416], W=[r_wl])
        dma("pool", wkrr[:, :, 64:80], w3[:, :, 400:416], W=[r_wl])
        dma("pool", wkrr[:, :, 80:96], w3[:, :, 384:400], W=[r_wl])

        def rms_scale(ps_list, rps, nch, N, scale):
            for c2 in range(nch):
                act(sqb[:, c2, 0:N], ps_list[c2], AF.Square, R=[rps[c2]], W=[r_sqb])
            pss = banks[7]
            for c2 in range(nch):
                mm(pss[:, 0:N], ones_bf[:, :], sqb[:, c2, 0:N], start=(c2 == 0), stop=(c2 == nch - 1), R=[r_sqb, r_const], W=[bR[7]])
            ts("dve", rr_t[:, 0:N], pss[:, 0:N], scale, RMS_EPS, ALU.mult, ALU.add, R=[bR[7]], W=[r_rr])
            act(rr_t[:, 0:N], rr_t[:, 0:N], AF.Sqrt, R=[r_rr], W=[r_rr])
            recip(rr_t[:, 0:N], rr_t[:, 0:N], R=[r_rr], W=[r_rr])

        def lat_compute(blk):
            isctx = blk < 0
            N = CTX if isctx else 512
            koff = 0 if isctx else CTX + blk * 512
            pk = banks[1]
            for k in range(8):
                mm(pk[:, 0:N], wkv[:, k, :], uT[:, k, 0:N], start=(k == 0), stop=(k == 7), R=[r_wl, r_uT], W=[bR[1]])
            rms_scale([pk[:, 0:N]], [bR[1]], 1, N, 1.0 / 128)
            tt("dve", kvnT[:, koff:koff + N], pk[:, 0:N], rr_t[:, 0:N], ALU.mult, R=[bR[1], r_rr], W=[r_kvn])
            p1 = banks[2]
            for k in range(8):
                mm(p1[0:96, 0:N], wkr[:, k, :], uT[:, k, 0:N], start=(k == 0), stop=(k == 7), R=[r_wl, r_uT], W=[bR[2]])
            if isctx:
                cp("act", krT[64:96, koff:koff + N], p1[64:96, 0:N], R=[bR[2]], W=[r_kr])
            else:
                p2 = banks[3]
                for k in range(8):
                    mm(p2[0:96, 0:N], wkrr[:, k, :], uT[:, k, 0:N], start=(k == 0), stop=(k == 7), R=[r_wl, r_uT], W=[bR[3]])
                dma("sp", cqb[64:96, :], ropec_d[64:96, blk * 512:(blk + 1) * 512], W=[r_rope])
                dma("sp", sqb_t[64:96, :], ropes_d[64:96, blk * 512:(blk + 1) * 512], W=[r_rope])
                tt("dve", t1[64:96, :], p1[64:96, 0:N], cqb[64:96, :], ALU.mult, R=[bR[2], r_rope], W=[r_t1])
                tt("dve", t2[64:96, :], p2[64:96, 0:N], sqb_t[64:96, :], ALU.mult, R=[bR[3], r_rope], W=[r_t2])
                tt("pool", krT[64:96, koff:koff + N], t1[64:96, :], t2[64:96, :], ALU.add, R=[r_t1, r_t2], W=[r_kr])
                pqs = [banks[4], banks[5]]
                for c2 in range(2):
                    for k in range(8):
                        mm(pqs[c2][:, :], wq[:, k, c2 * 128:(c2 + 1) * 128], uT[:, k, :], start=(k == 0), stop=(k == 7),
                           R=[r_wl, r_uT], W=[bR[4 + c2]])
                rms_scale([pqs[0][:, :], pqs[1][:, :]], [bR[4], bR[5]], 2, 512, 1.0 / 256)
                for c2 in range(2):
                    tt("dve", qnT[:, c2, blk * 512:(blk + 1) * 512], pqs[c2][:, :], rr_t[:, :], ALU.mult, R=[bR[4 + c2], r_rr], W=[r_qn[blk]])

        def na_project(blk):
            isctx = blk < 0
            N = CTX if isctx else 512
            ntl = N // 128
            slots = [RING + t for t in range(ntl)] if isctx else [(blk * 4 + t) % RING for t in range(ntl)]
            par = blk % 2
            for c in range(4):
                if not isctx:
                    pq = banks[1 + c % 2]
                    for k in range(8):
                        mm(pq[:, 0:N], wna[:, k, c * 128:(c + 1) * 128], uT[:, k, 0:N], start=(k == 0), stop=(k == 7),
                           R=[r_wna, r_uT], W=[bR[1 + c % 2]])
                    act(nqT[par][:, c, :], pq[:, 0:N], AF.Identity, R=[bR[1 + c % 2]], W=[r_nq[par]], scale=0.125)
                pk = banks[3 + c % 2]
                for k in range(8):
                    mm(pk[:, 0:N], wna[:, k, 512 + c * 128:512 + (c + 1) * 128], uT[:, k, 0:N], start=(k == 0), stop=(k == 7),
                       R=[r_wna, r_uT], W=[bR[3 + c % 2]])
                for t in range(ntl):
                    s = slots[t]
                    cp("dve", nkT[:, c, s * 128:(s + 1) * 128], pk[:, t * 128:(t + 1) * 128], R=[bR[3 + c % 2]], W=[r_nk[s]])
            for t in range(ntl):
                s = slots[t]
                pv = banks[1 + t % 2]
                for k in range(8):
                    mm(pv[:, :], uT[:, k, t * 128:(t + 1) * 128], wna[:, k, 1024:1536], start=(k == 0), stop=(k == 7),
                       R=[r_wna, r_uT], W=[bR[1 + t % 2]])
                cp("act", nva[:, s, :, 0:64], pv[:, :].rearrange("p (h c) -> p h c", c=64), R=[bR[1 + t % 2]], W=[r_nv[s]])
            lat_compute(blk)

        na_i = [0]
        na_pend = []

        def na_rows(blk, bg=None):
            par = blk % 2
            yb = yblk[par]
            for rr in range(8):
                qr = blk * 8 + rr
                r0 = min(max(qr - 4, 0), 56)
                kt0 = r0 // 2
                nt = 4 if r0 % 2 == 0 else 5
                if 4 <= qr <= 59:
                    tbv = tb01
                    tb0 = 4 * (r0 % 2)
                    r_tb = r_tb01
                else:
                    v = 2 + qr if qr < 4 else 6 + (qr - 60)
                    for half in range(2):
                        for hh in range(NH):
                            dma("pool", tbe[half * 64:(half + 1) * 64, hh, :, :], natab_d[v][hh, 0:4].rearrange("c q k -> q c k"), W=[r_tbe])
                    tbv = tbe
                    tb0 = 0
                    r_tb = r_tbe
                qoff = rr * 64
                tiles = [(kt0 + ci) % RING for ci in range(nt)] + [RING, RING + 1]
                ncol = len(tiles) * 64
                for hp in range(4):
                    k2 = na_i[0] % 2
                    na_i[0] += 1
                    psb = (5, 6) if k2 == 0 else (1, 2)
                    pob = (7, 0) if k2 == 0 else (3, 4)
                    c = hp
                    for ci, s in enumerate(tiles):
                        loc = ci < nt
                        for hh in range(2):
                            pb = hh * 64
                            mm(banks[psb[hh]][:, ci * 64:(ci + 1) * 64], nkT[pb:pb + 64, c, s * 128:(s + 1) * 128],
                               nqT[par][pb:pb + 64, c, qoff:qoff + 64], start=True, stop=not loc,
                               R=[r_nk[s], r_nq[par]], W=[bR[psb[hh]]])
                        if loc:
                            for hh in range(2):
                                pb = hh * 64
                                mm(banks[psb[hh]][:, ci * 64:(ci + 1) * 64], tbv[pb:pb + 64, 2 * hp + hh, tb0 + ci, :], ident_bf[pb:pb + 64, pb:pb + 64],
                                   start=False, stop=True, R=[r_tb, r_const], W=[bR[psb[hh]]])
                    if na_pend:
                        na_pend.pop()()

                    def fin(k2=k2, hp=hp, c=c, tiles=tiles, ncol=ncol, qoff=qoff, yb=yb, par=par, psb=psb, pob=pob):
                        for hh in range(2):
                            i = 2 * k2 + hh
                            h = 2 * hp + hh
                            pb = hh * 64
                            ps = banks[psb[hh]]
                            act(pT[i][:, 0:ncol], ps[:, 0:ncol], AF.Exp, R=[bR[psb[hh]]], W=[r_pT[i]])
                        for hh in range(2):
                            i = 2 * k2 + hh
                            h = 2 * hp + hh
                            pb = hh * 64
                            po = banks[pob[hh]]
                            rpo = bR[pob[hh]]
                            for ci, s in enumerate(tiles):
                                mm(po[:, 0:64], nva[:, s, h, :], pT[i][:, ci * 64:(ci + 1) * 64], start=(ci == 0), stop=(ci == len(tiles) - 1),
                                   R=[r_nv[s], r_pT[i]], W=[rpo])
                            recip(rcn[i][0:64, :], po[64:128, 0:64], R=[rpo], W=[r_rcn[i]])
                            tt("dve", yb[pb:pb + 64, c, qoff:qoff + 64], po[0:64, 0:64], rcn[i][0:64, :], ALU.mult, R=[rpo, r_rcn[i]], W=[r_yblk[par]])
                    na_pend.append(fin)
                    if bg is not None:
                        next(bg, None)
                        next(bg, None)
            if na_pend:
                na_pend.pop()()

        def na_store(blk):
            dma("sp", yna_d.rearrange("(c p) t -> p c t", p=128)[:, :, blk * 512:(blk + 1) * 512], yblk[blk % 2][:, :, :],
                R=[r_yblk[blk % 2]], W=[r_yna[blk]])

        def ln0_bg(blk):
            for t0 in (0, 2):
                gens = [ln0_tile(x_d, blk * 512, t, 0, xts, r_xts, xh, r_xh, uT, r_uT, True, 4 + t % 2, 0, skew=4) for t in (t0, t0 + 1)]
                while gens:
                    for g in list(gens):
                        try:
                            next(g)
                        except StopIteration:
                            gens.remove(g)
                        yield

        ln0_block(ctx_d, 0, 2, 1, xts, r_xts, xh, r_xh, uT, r_uT)
        na_project(-1)
        ln0_block(x_d, 0, 4, 0, xts, r_xts, xh, r_xh, uT, r_uT)
        for v in range(2):
            for half in range(2):
                for hh in range(NH):
                    ntv = 4 if v == 0 else 5
                    dma("pool", tb01[half * 64:(half + 1) * 64, hh, 4 * v:4 * v + ntv, :], natab_d[v][hh, 0:ntv].rearrange("c q k -> q c k"), W=[r_tb01])
        for blk in range(8):
            na_project(blk)
            bg = ln0_bg(blk + 1) if blk + 1 < 8 else None
            if blk >= 2:
                na_store(blk - 2)
            if blk >= 1:
                na_rows(blk - 1, bg)
            if bg is not None:
                for _ in bg:
                    pass
        na_store(6)
        na_rows(7)
        na_store(7)

        P.barrier()
        cur[0] = AW - 8192
        y_mlaT = alloc([128, 4, SEQ], BF); r_ymla = [Res() for _ in range(8)]
        lim[0] = AW - 8192
        cur[0] = LAT_END
        wuq = alloc([128, 2, 768], BF); wuqr = alloc([128, 2, NH, 96], BF); wukv = alloc([128, 1024], BF); r_wu = Res()
        stg = alloc([128, 2, 768]); r_stg = Res()
        qg_sb = alloc([128, 2]); kvg_sb = alloc([128, 1]); r_g = Res()
        kh = [alloc([128, NKEY], BF) for _ in range(2)]; r_kh = [Res(), Res()]
        vm = [alloc([128, 34, 128], BF) for _ in range(2)]; r_vm = [Res(), Res()]
        qh = [alloc([128, 512], BF) for _ in range(2)]; r_qh = [Res(), Res()]
        cq2 = [alloc([128, 512]) for _ in range(2)]; sq2 = [alloc([128, 512]) for _ in range(2)]; r_rp = [Res(), Res()]
        t1 = alloc([128, 512]); t2 = alloc([128, 512]); r_t1 = Res(); r_t2 = Res()
        pTm = [alloc([128, 512], BF) for _ in range(3)]; r_pTm = [Res() for _ in range(3)]
        rcm = [alloc([128, 512]) for _ in range(2)]; r_rcm = [Res(), Res()]

        dma("sp", qg_sb[:, :], qg_d[:, :], W=[r_g])
        dma("sp", kvg_sb[:, :], kvg_d[:, :], W=[r_g])
        dma("sp", stg[:, :, :], wuq_d.rearrange("(k p) n -> p k n", p=128), W=[r_stg])
        for c2 in range(2):
            ts("dve", wuq[:, c2, :], stg[:, c2, :], qg_sb[:, c2:c2 + 1], None, ALU.mult, R=[r_stg, r_g], W=[r_wu])
        memset("pool", wuqr[:, :, :, 0:64], 0.0, W=[r_wu])
        wuq4 = wuq.rearrange("p k (h c) -> p k h c", c=96)
        for c2 in range(2):
            cp("dve", wuqr[:, c2, :, 64:80], wuq4[:, c2, :, 80:96], R=[r_wu], W=[r_wu])
            cp("dve", wuqr[:, c2, :, 80:96], wuq4[:, c2, :, 64:80], R=[r_wu], W=[r_wu])
        stg_kv = stg.rearrange("p a b -> p (a b)")[:, 0:1024]; r_stgkv = r_stg
        dma("sp", stg_kv[:, :], wukv_d[:, :], W=[r_stgkv])
        ts("dve", wukv[:, :], stg_kv[:, :], kvg_sb[:, 0:1], None, ALU.mult, R=[r_stgkv, r_g], W=[r_wu])
        for i in range(2):
            memset("pool", vm[i][:, :, 64:128], 1.0, W=[r_vm[i]])

        def build_kv(h):
            hp = h % 2
            for j in range(9):
                n = 512 if j < 8 else 256
                pk = banks[6]
                mm(pk[0:64, 0:n], wukv[:, h * 128:h * 128 + 64], kvnT[:, j * 512:j * 512 + n], R=[r_wu, r_kvn], W=[bR[6]])
                cp("dve", kh[hp][0:64, j * 512:j * 512 + n], pk[0:64, 0:n], R=[bR[6]], W=[r_kh[hp]])
                yield
            cp("pool", kh[hp][64:96, :], krT[64:96, :], R=[r_kr], W=[r_kh[hp]])
            for g0 in range(0, 34, 8):
                gn = min(8, 34 - g0)
                pv = banks[7]
                for kt in range(g0, g0 + gn):
                    mm(pv[:, (kt - g0) * 64:(kt - g0 + 1) * 64], kvnT[:, kt * 128:(kt + 1) * 128], wukv[:, h * 128 + 64:h * 128 + 128],
                       R=[r_wu, r_kvn], W=[bR[7]])
                cp("dve", vm[hp][:, g0:g0 + gn, 0:64], pv[:, 0:gn * 64].rearrange("p (a b) -> p a b", b=64), R=[bR[7]], W=[r_vm[hp]])
                yield

        def build_q(it):
            h, qb = it // 8, it % 8
            ip = it % 2
            dma("sp", cq2[ip][0:96, :], ropec_d[:, qb * 512:(qb + 1) * 512], W=[r_rp[ip]])
            dma("sp", sq2[ip][0:96, :], ropes_d[:, qb * 512:(qb + 1) * 512], W=[r_rp[ip]])
            yield
            p1 = banks[5]
            rb1 = bR[5]
            for c2 in range(2):
                mm(p1[0:96, :], wuq[:, c2, h * 96:(h + 1) * 96], qnT[:, c2, qb * 512:(qb + 1) * 512], start=(c2 == 0), stop=(c2 == 1),
                   R=[r_wu, r_qn[qb]], W=[rb1])
            tt("dve", t1[0:96, :], p1[0:96, :], cq2[ip][0:96, :], ALU.mult, R=[rb1, r_rp[ip]], W=[r_t1])
            yield
            for c2 in range(2):
                mm(p1[0:96, :], wuqr[:, c2, h, :], qnT[:, c2, qb * 512:(qb + 1) * 512], start=(c2 == 0), stop=(c2 == 1),
                   R=[r_wu, r_qn[qb]], W=[rb1])
            tt("dve", t2[0:96, :], p1[0:96, :], sq2[ip][0:96, :], ALU.mult, R=[rb1, r_rp[ip]], W=[r_t2])
            yield
            tt("pool", qh[ip][0:96, :], t1[0:96, :], t2[0:96, :], ALU.add, R=[r_t1, r_t2], W=[r_qh[ip]])

        def attention(it, bgs):
            h, qb = it // 8, it % 8
            ip = it % 2
            hp = h % 2
            po = banks[3 + ip]

            def s_issue(kc):
                si = kc % 3
                mm(banks[si][:, :], kh[hp][0:96, kc * 128:(kc + 1) * 128], qh[ip][0:96, :], R=[r_kh[hp], r_qh[ip]], W=[bR[si]])
            for kc in range(3):
                s_issue(kc)
            for kc in range(34):
                si = kc % 3
                act(pTm[si][:, :], banks[si][:, :], AF.Exp, R=[bR[si]], W=[r_pTm[si]], scale=MLA_SCALE)
                mm(po[:, :], vm[hp][:, kc, :], pTm[si][:, :], start=(kc == 0), stop=(kc == 33), R=[r_vm[hp], r_pTm[si]], W=[bR[3 + ip]])
                if kc + 3 < 34:
                    s_issue(kc + 3)
                if kc % 4 == 1:
                    for g in bgs:
                        next(g, None)
                if kc == 17:
                    next(modgen, None)
            recip(rcm[ip][0:64, :], po[64:128, :], R=[bR[3 + ip]], W=[r_rcm[ip]])
            tt("dve", y_mlaT[hp * 64:hp * 64 + 64, h // 2, qb * 512:(qb + 1) * 512], po[0:64, :], rcm[ip][0:64, :], ALU.mult,
               R=[bR[3 + ip], r_rcm[ip]], W=[r_ymla[qb]])

        cc2 = alloc([128, 8, 2]); scc2 = alloc([128, 8, 2]); r_cc2 = Res()
        screp = alloc([128, 8, 128]); r_screp = Res()
        wmc = [alloc([128, 8, 256]) for _ in range(2)]; r_wmc = [Res(), Res()]
        bmb = [alloc([128, 256]) for _ in range(2)]; r_bmb = [Res(), Res()]
        g2_t = alloc([128, D]); r_g2 = Res()

        def modbc_gen():
            dma("sp", cc2[:, :, :], cc_d[:, :, :], W=[r_cc2])
            yield
            act(scc2[:, :, :], cc2[:, :, :], AF.Silu, R=[r_cc2], W=[r_cc2])
            for k in range(8):
                ts("dve", screp[:, k, :], ones_f[:, :], scc2[:, k, 0:1], None, ALU.mult, R=[r_cc2, r_const], W=[r_screp])
            yield
            dsts = [g1_bc, sh2_bc, sc2p1_bc, g2_t]
            rds = [r_g1, r_bc, r_bc, r_g2]
            for jj in range(16):
                f0 = 2048 + jj * 256
                reg = (jj * 256) // 1024
                c0 = (jj * 256) % 1024
                wb = wmc[jj % 2]; bb = bmb[jj % 2]
                dma("sp", wb[:, :, :], wmod_d[:, f0:f0 + 256].rearrange("(k p) n -> p k n", p=128), W=[r_wmc[jj % 2]])
                dma("sp", bb[:, :], bcast_part(bmodrow_d[0:1, f0:f0 + 256], 128), W=[r_bmb[jj % 2]])
                yield
                pb = banks[5]
                for k in range(8):
                    mm(pb[:, 0:256], screp[:, k, :], wb[:, k, :], start=(k == 0), stop=(k == 7), R=[r_screp, r_wmc[jj % 2]], W=[bR[5]])
                dst = dsts[reg][:, c0:c0 + 256]
                tt("dve", dst, pb[:, 0:256], bb[:, :], ALU.add, R=[bR[5], r_bmb[jj % 2]], W=[rds[reg]])
                if reg == 2:
                    ts("dve", dst, dst, 1.0, None, ALU.add, R=[rds[reg]], W=[rds[reg]])
                elif reg == 3:
                    ts("dve", dst, dst, 1.0 / ALPHA, None, ALU.mult, R=[rds[reg]], W=[rds[reg]])
                yield
            dma("sp", g2_d[:, :], g2_t[:, :], R=[r_g2], W=[r_g2d])

        modgen = modbc_gen()
        for _ in build_kv(0):
            pass
        for _ in build_q(0):
            pass
        kvgen = None
        for it in range(64):
            if it % 8 == 0:
                if kvgen is not None:
                    for _ in kvgen:
                        pass
                kvgen = build_kv(it // 8 + 1) if it // 8 + 1 < NH else None
            qgen = build_q(it + 1) if it + 1 < 64 else None
            attention(it, [g for g in (qgen, kvgen) if g is not None])
            if qgen is not None:
                for _ in qgen:
                    pass
        for _ in modgen:
            pass

        P.barrier()
        cur[0] = PERSIST
        wg_in = alloc([128, 8, 2048], BF); r_wgin = Res()
        wpm = alloc([128, 4, D], BF); wpn = alloc([128, 4, D], BF); r_wp = Res()
        wout = alloc([128, 8, D], BF); r_wout = Res()
        wr_sb = alloc([128, 8, NE]); r_wr = Res()
        xts = [alloc([128, D]) for _ in range(2)]; r_xts = [Res() for _ in range(2)]
        xh = [alloc([128, D], BF) for _ in range(2)]; r_xh = [Res(), Res()]
        uTs = [alloc([128, 8, 512], BF) for _ in range(2)]; r_uTs = [[Res() for _ in range(4)] for _ in range(2)]
        mTs = [alloc([128, 8, 512], BF) for _ in range(2)]; r_mTs = [Res(), Res()]
        ynab = alloc([128, 4, 512], BF); r_ynab = Res()
        sig_all = g1_bc
        sig = [sig_all[:, 0:512], sig_all[:, 512:1024]]; r_sig = [r_g1, r_g1]
        tAB = alloc([128, 1024]); tA = tAB[:, 0:512]; tB = tAB[:, 512:1024]; r_tA = Res(); r_tB = r_tA
        UTMP[0] = alloc([128, 8, 128]); UTMP[1] = Res()
        Xs = [alloc([128, D]) for _ in range(4)]; r_Xs = [Res() for _ in range(4)]
        u2bs = [alloc([128, D], BF) for _ in range(2)]; r_u2bs = [Res(), Res()]
        u2Ts = [alloc([128, 8, 128]) for _ in range(2)]; r_u2Ts = [Res(), Res()]
        sms = [alloc([128, 8]) for _ in range(2)]; r_sms = [Res(), Res()]
        exs = [alloc([128, NE]) for _ in range(2)]; r_exs = [Res(), Res()]
        r_u2d_t = [Res() for _ in range(32)]; r_accd_t = [Res() for _ in range(32)]

        dma("pool", wg_in[:, :, :], w3[:, :, 1952:4000], W=[r_wgin])
        dma("pool", wpm[:, :, :], wpm_d.rearrange("(k p) n -> p k n", p=128), W=[r_wp])
        dma("pool", wpn[:, :, :], wpn_d.rearrange("(k p) n -> p k n", p=128), W=[r_wp])
        dma("sp", wr_sb[:, :, :], wr_d.rearrange("(k p) n -> p k n", p=128), W=[r_wr])
        wo3 = wout_d.rearrange("(k p) n -> p k n", p=128)
        for k in range(8):
            dma("sp", xts[k % 2][:, :], wo3[:, k, :], W=[r_xts[k % 2]])
            tt("dve" if k % 2 == 0 else "pool", wout[:, k, :], xts[k % 2][:, :], g1_bc[:, :], ALU.mult, R=[r_xts[k % 2], r_g1], W=[r_wout])

        def dcgen(blk):
            uT = uTs[blk % 2]; r_uT = r_uTs[blk % 2]; mT = mTs[blk % 2]; r_mT = r_mTs[blk % 2]
            dma("sp", ynab[:, :, :], yna_d.rearrange("(c p) t -> p c t", p=128)[:, :, blk * 512:(blk + 1) * 512], R=[r_yna[blk]], W=[r_ynab])
            yield
            for dc in range(8):
                for gi in range(2):
                    pg = banks[1 + gi]
                    for k in range(8):
                        mm(pg[:, :], wg_in[:, k, gi * 1024 + dc * 128:gi * 1024 + (dc + 1) * 128], uT[:, k, :], start=(k == 0), stop=(k == 7),
                           R=[r_wgin, r_uT], W=[bR[1 + gi]])
                    act(sig[gi][:, :], pg[:, :], AF.Sigmoid, R=[bR[1 + gi]], W=[r_sig[gi]])
                    yield
                pA = banks[3]; pB = banks[4]
                for c in range(4):
                    mm(pA[:, :], wpm[:, c, dc * 128:(dc + 1) * 128], y_mlaT[:, c, blk * 512:(blk + 1) * 512], start=(c == 0), stop=(c == 3),
                       R=[r_wp, r_ymla[blk]], W=[bR[3]])
                for c in range(4):
                    mm(pB[:, :], wpn[:, c, dc * 128:(dc + 1) * 128], ynab[:, c, :], start=(c == 0), stop=(c == 3), R=[r_wp, r_ynab], W=[bR[4]])
                tt("dve", tA[:, :], pA[:, :], sig[0][:, :], ALU.mult, R=[bR[3], r_sig[0]], W=[r_tA])
                tt("dve", tB[:, :], pB[:, :], sig[1][:, :], ALU.mult, R=[bR[4], r_sig[1]], W=[r_tB])
                tt("pool", mT[:, dc, :], tA[:, :], tB[:, :], ALU.add, R=[r_tA, r_tB], W=[r_mT])
                yield

        def tchain(blk, t):
            T = blk * 4 + t
            p = t % 2
            mT = mTs[blk % 2]; r_mT = r_mTs[blk % 2]
            X = Xs[t]; rX = r_Xs[t]
            u2b = u2bs[p]; r_u2b = r_u2bs[p]; u2T = u2Ts[p]; r_u2T = r_u2Ts[p]; sm = sms[p]; r_sm = r_sms[p]; ex = exs[p]; r_ex = r_exs[p]
            l0 = 2 * t; l1 = 2 * t + 1
            dma("sp", X[:, :], x_d[T * 128:(T + 1) * 128, :], W=[rX])
            yield
            for hf in range(2):
                bi = 5 + hf
                pm_ = banks[bi]
                for k in range(8):
                    mm(pm_[:, :], mT[:, k, t * 128:(t + 1) * 128], wout[:, k, hf * 512:(hf + 1) * 512], start=(k == 0), stop=(k == 7),
                       R=[r_mT, r_wout], W=[bR[bi]])
                stt("dve", X[:, hf * 512:(hf + 1) * 512], X[:, hf * 512:(hf + 1) * 512], ALPHA, pm_[:, :], ALU.mult, ALU.add,
                    R=[rX, bR[bi]], W=[rX])
                yield
            yield from ln_stats_g(X, rX, l0)
            stt("dve", X[:, :], X[:, :], mv[l0][:, 0:1], ln1g_bc[:, :], ALU.subtract, ALU.mult, R=[rX, r_ln[l0], r_bc], W=[rX])
            stt("dve", X[:, :], X[:, :], rstd[l0][:, 0:1], ln1b_bc[:, :], ALU.mult, ALU.add, R=[rX, r_ln[l0], r_bc], W=[rX])
            yield
            dma("sp", acc_d[T * 128:(T + 1) * 128, :], X[:, :], R=[rX], W=[r_accd_t[T]])
            yield from ln_stats_g(X, rX, l1)
            stt("dve", X[:, :], X[:, :], mv[l1][:, 0:1], sc2p1_bc[:, :], ALU.subtract, ALU.mult, R=[rX, r_ln[l1], r_bc], W=[rX])
            stt("dve", X[:, :], X[:, :], rstd[l1][:, 0:1], sh2_bc[:, :], ALU.mult, ALU.add, R=[rX, r_ln[l1], r_bc], W=[rX])
            yield
            cp("act", u2b[:, :], X[:, :], R=[rX], W=[r_u2b])
            dma("sp", u2_d[T * 128:(T + 1) * 128, :], u2b[:, :], R=[r_u2b], W=[r_u2d_t[T]])
            pt_ = banks[7]
            for hf in range(2):
                for q4 in range(4):
                    fc = hf * 4 + q4
                    tr(pt_[:, q4 * 128:(q4 + 1) * 128], X[:, fc * 128:(fc + 1) * 128], ident_f[:, :], R=[rX, r_const], W=[bR[7]])
                cp("act" if hf == 0 else "dve", u2T[:, hf * 4:(hf + 1) * 4, :], pt_[:, :].rearrange("p (a b) -> p a b", b=128), R=[bR[7]], W=[r_u2T])
                yield
            pl = banks[7]
            for k in range(8):
                mm(pl[:, 0:NE], u2T[:, k, :], wr_sb[:, k, :], start=(k == 0), stop=(k == 7), R=[r_u2T, r_wr], W=[bR[7]])
            P.op("dve", lambda e, pl=pl, sm=sm: e.reduce_max(out=sm[:, 0:1], in_=pl[:, 0:NE], axis=AX.X), [bR[7]], [r_sm])
            ts("dve", sm[:, 1:2], sm[:, 0:1], -1.0, None, ALU.mult, R=[r_sm], W=[r_sm])
            act(ex[:, :], pl[:, 0:NE], AF.Exp, R=[bR[7], r_sm], W=[r_ex, r_sm], bias=sm[:, 1:2], accum_out=sm[:, 2:3])
            recip(sm[:, 3:4], sm[:, 2:3], R=[r_sm], W=[r_sm])
            ts("dve", aff[:, T, :], ex[:, :], sm[:, 3:4], None, ALU.mult, R=[r_ex, r_sm], W=[r_aff])

        def ln0_items(blk, first_tick):
            uT = uTs[blk % 2]; r_uT = r_uTs[blk % 2]
            return [(first_tick + 7 * (t // 2), ln0_tile(x_d, blk * 512, t, 0, xts, r_xts, xh, r_xh, uT, r_uT, True, 8 + t % 2, 0)) for t in range(4)]

        ln0_block(x_d, 0, 4, 0, xts, r_xts, xh, r_xh, uTs[0], r_uTs[0])
        run_stag([(0, dcgen(0))] + ln0_items(1, 1))
        STG = 4
        for blk in range(8):
            items = [(0, tchain(blk, 0)), (0, tchain(blk, 1)), (STG, tchain(blk, 2)), (STG, tchain(blk, 3))]
            if blk + 1 < 8:
                items.append((0, dcgen(blk + 1)))
            if blk + 2 < 8:
                items += ln0_items(blk + 2, 1)
            run_stag(items)

        P.barrier()
        lim[0] = AW
        r_u2d = Res("u2d"); r_accd = Res("accd")
        cur[0] = PERSIST
        g2_bc = alloc([128, D]); ln2g_bc = alloc([128, D]); ln2b_bc = alloc([128, D]); r_bc2 = Res()
        dma("sp", g2_bc[:, :], g2_d[:, :], R=[r_g2d], W=[r_bc2])
        dma("sp", ln2g_bc[:, :], bcast_part(lnv_d[2:3, :], 128), W=[r_bc2])
        dma("sp", ln2b_bc[:, :], bcast_part(lnv_d[3:4, :], 128), W=[r_bc2])
        MOE_BASE = cur[0]
        wring = [alloc([128, 8, D], BF) for _ in range(4)]; r_wring = [Res() for _ in range(4)]
        lo = alloc([128, NE]); hi = alloc([128, NE]); mid = alloc([128, NE]); r_th = Res()
        cmpm = alloc([128, 32, NE]); r_cmp = Res()
        cntp = alloc([128, NE]); r_cntp = Res()
        ge = alloc([128, NE]); dlo = alloc([128, NE]); dhi = alloc([128, NE]); r_ge = Res()
        base = alloc([128, NE]); r_base = Res()
        cs = alloc([128, NE, 32]); posm = alloc([128, NE, 32]); r_pos = Res()
        rcb = alloc([128, NE, 32, 6], BF); r_rc2 = Res()
        gsp = alloc([128, NE, 32]); gsb = alloc([128, NE, 32], BF); gsf = alloc([128, NE, 32]); r_gs = Res()
        tj_i = alloc([128, 32], I32); tp_i = alloc([128, 32], I32)
        tokid_i = alloc([128, 32], I32); tokid = alloc([128, 32]); iota_i = alloc([128, 512], I32); iota_s = alloc([128, 512]); r_io = Res()
        oh = [alloc([128, 512], BF) for _ in range(4)]; r_oh = [Res() for _ in range(4)]
        cmp_sb = alloc([128, 512]); r_cmps = Res()
        ig5 = alloc([128, 4, 6]); r_ig5 = Res()
        ig = alloc([128, NE, 4, 2]); r_ig = [Res() for _ in range(NE)]
        idx_i = alloc([128, NE, 4], I32); r_idx = [Res() for _ in range(NE)]
        xe = alloc([128, 4, D], BF); r_xe = Res()
        xeT = alloc([128, 8, 512], BF); r_xeT = Res()
        hT = alloc([128, 8, 512], BF); r_hT = Res()
        sg = [alloc([128, 512]) for _ in range(2)]; r_sg = [Res(), Res()]
        ye = [alloc([128, D]) for _ in range(4)]; r_ye = [Res() for _ in range(4)]
        r_acc_e = [[Res() for _ in range(4)] for _ in range(NE)]

        wsrc = []
        for e_ in range(NE):
            wsrc += [wg_d[e_], wu_d[e_], wd_d[e_]]
        wload = [0]

        def load_next_w():
            i = wload[0]
            if i >= len(wsrc):
                return
            wload[0] += 1
            dma("pool", wring[i % 4][:, :, :], wsrc[i].rearrange("(k p) n -> p k n", p=128), W=[r_wring[i % 4]])
        for _ in range(4):
            load_next_w()

        P.op("pool", lambda e: e.iota(tj_i[:, :], pattern=[[1, 32]], base=0, channel_multiplier=0), (), [r_io])
        P.op("pool", lambda e: e.iota(tp_i[:, :], pattern=[[0, 32]], base=0, channel_multiplier=1), (), [r_io])
        P.op("pool", lambda e: e.iota(iota_i[:, :], pattern=[[1, 512]], base=0, channel_multiplier=0), (), [r_io])
        cp("dve", iota_s[:, :], iota_i[:, :], R=[r_io], W=[r_io])
        memset("dve", lo[:, :], 0.0, W=[r_th])
        memset("dve", hi[:, :], 1.0, W=[r_th])
        aff_ej = aff.rearrange("p j e -> p e j")

        def count_ge(thr):
            tt("dve", cmpm[:, :, :], aff[:, :, :], bcast_mid(thr[:, :], 32), ALU.is_ge, R=[r_aff, r_th], W=[r_cmp])
            P.op("dve", lambda e: e.tensor_reduce(out=cntp[:, :], in_=cmpm.rearrange("p j e -> p e j"), axis=AX.X, op=ALU.add), [r_cmp], [r_cntp])
        for it in range(NBIS):
            hw = 0.5 ** (it + 1)
            ts("dve", mid[:, :], lo[:, :], hw, None, ALU.add, R=[r_th], W=[r_th])
            count_ge(mid)
            pc = banks[it % 2]
            mm(pc[:, 0:NE], ones_f[:, :], cntp[:, :], R=[r_cntp, r_const], W=[bR[it % 2]])
            ts("dve", ge[:, :], pc[:, 0:NE], CAP - 0.5, hw, ALU.is_gt, ALU.mult, R=[bR[it % 2]], W=[r_ge])
            tt("dve", lo[:, :], lo[:, :], ge[:, :], ALU.add, R=[r_ge, r_th], W=[r_th])
        count_ge(lo)
        pbse = banks[2]
        mm(pbse[:, 0:NE], tri_f[:, :], cntp[:, :], R=[r_cntp, r_const], W=[bR[2]])
        cp("dve", base[:, :], pbse[:, 0:NE], R=[bR[2]], W=[r_base])
        m_ej = cmpm.rearrange("p j e -> p e j")
        for e_ in range(NE):
            P.op("dve", lambda e, e_=e_: e.tensor_tensor_scan(out=cs[:, e_, :], data0=ones_f[:, 0:32], data1=m_ej[:, e_, :], initial=0.0,
                                                              op0=ALU.mult, op1=ALU.add), [r_cmp, r_const], [r_pos])
            stt("dve", posm[:, e_, :], cs[:, e_, :], base[:, e_:e_ + 1], m_ej[:, e_, :], ALU.add, ALU.subtract, R=[r_pos, r_base, r_cmp], W=[r_pos])
            stt("dve", posm[:, e_, :], posm[:, e_, :], 1.0, m_ej[:, e_, :], ALU.add, ALU.mult, R=[r_pos, r_cmp], W=[r_pos])
            ts("dve", posm[:, e_, :], posm[:, e_, :], -1.0, None, ALU.add, R=[r_pos], W=[r_pos])
            cp("pool", rcb[:, e_, :, 0], tj_i[:, :], R=[r_io], W=[r_rc2])
            cp("pool", rcb[:, e_, :, 1], tp_i[:, :], R=[r_io], W=[r_rc2])
            tt("pool", gsp[:, e_, :], aff_ej[:, e_, :], m_ej[:, e_, :], ALU.mult, R=[r_aff, r_cmp], W=[r_gs])
        memset("pool", rcb[:, :, :, 5], 0.0, W=[r_rc2])
        for part in range(3):
            cp("dve", gsb[:, :, :], gsp[:, :, :], R=[r_gs], W=[r_gs])
            cp("dve", rcb[:, :, :, 2 + part], gsb[:, :, :], R=[r_gs], W=[r_rc2])
            if part < 2:
                cp("dve", gsf[:, :, :], gsb[:, :, :], R=[r_gs], W=[r_gs])
                tt("dve", gsp[:, :, :], gsp[:, :, :], gsf[:, :, :], ALU.subtract, R=[r_gs], W=[r_gs])

        def compact(e_):
            pc = banks[0]
            pend = []
            for j in range(32):
                o = oh[j % 4]
                ts("dve", o[:, :], iota_s[:, :], posm[:, e_, j:j + 1], None, ALU.is_equal, R=[r_io, r_pos], W=[r_oh[j % 4]])
                pend.append(j)
                if j % 2 == 1:
                    while len(pend) > 2:
                        jj = pend.pop(0)
                        mm(pc[0:6, :], rcb[:, e_, jj, :], oh[jj % 4][:, :], start=(jj == 0), stop=False, R=[r_rc2, r_oh[jj % 4]], W=[bR[0]])
                    yield
            while pend:
                jj = pend.pop(0)
                mm(pc[0:6, :], rcb[:, e_, jj, :], oh[jj % 4][:, :], start=(jj == 0), stop=(jj == 31), R=[r_rc2, r_oh[jj % 4]], W=[bR[0]])
            cp("dve", cmp_sb[0:6, :], pc[0:6, :], R=[bR[0]], W=[r_cmps])
            yield
            pt_ = banks[0]
            for s in range(4):
                tr(pt_[:, s * 6:(s + 1) * 6], cmp_sb[0:6, s * 128:(s + 1) * 128], ident_f[0:6, 0:6], R=[r_cmps, r_const], W=[bR[0]])
            cp("dve", ig5[:, :, :], pt_[:, 0:24].rearrange("p (a b) -> p a b", b=6), R=[bR[0]], W=[r_ig5])
            stt("dve", ig[:, e_, :, 0], ig5[:, :, 0], 128.0, ig5[:, :, 1], ALU.mult, ALU.add, R=[r_ig5], W=[r_ig[e_]])
            tt("dve", ig[:, e_, :, 1], ig5[:, :, 2], ig5[:, :, 3], ALU.add, R=[r_ig5], W=[r_ig[e_]])
            tt("dve", ig[:, e_, :, 1], ig[:, e_, :, 1], ig5[:, :, 4], ALU.add, R=[r_ig5, r_ig[e_]], W=[r_ig[e_]])
            cp("dve", idx_i[:, e_, :], ig[:, e_, :, 0], R=[r_ig[e_]], W=[r_idx[e_]])
            yield

        def gather(e_):
            for s in range(4):
                P.dma("pool", lambda e, s=s: e.indirect_dma_start(out=xe[:, s, :], out_offset=None, in_=u2_d[:, :],
                                                                  in_offset=bass.IndirectOffsetOnAxis(ap=idx_i[:, e_, s:s + 1], axis=0)),
                      [r_idx[e_], r_u2d], [r_xe])

        ptb_m = banks[2].bitcast(BF).rearrange("p (a b) -> p a b", b=128)

        def xpose(s):
            for dk in range(8):
                tr(ptb_m[:, dk, :], xe[:, s, dk * 128:(dk + 1) * 128], ident_bf[:, :], R=[r_xe, r_const], W=[bR[2]])
            cp("dve" if s % 2 == 0 else "act", xeT[:, :, s * 128:(s + 1) * 128], ptb_m[:, :, :], R=[bR[2]], W=[r_xeT])

        def expert(e_, bg):
            def tick():
                if bg is not None:
                    next(bg, None)
            wi = e_ * 3
            Wg = wring[wi % 4]; Wu = wring[(wi + 1) % 4]; Wd = wring[(wi + 2) % 4]
            rWg = r_wring[wi % 4]; rWu = r_wring[(wi + 1) % 4]; rWd = r_wring[(wi + 2) % 4]
            for fc in range(8):
                pG = banks[3 + fc % 2]; pU = banks[5 + fc % 2]
                for dk in range(8):
                    mm(pG[:, :], Wg[:, dk, fc * 128:(fc + 1) * 128], xeT[:, dk, :], start=(dk == 0), stop=(dk == 7), R=[rWg, r_xeT], W=[bR[3 + fc % 2]])
                for dk in range(8):
                    mm(pU[:, :], Wu[:, dk, fc * 128:(fc + 1) * 128], xeT[:, dk, :], start=(dk == 0), stop=(dk == 7), R=[rWu, r_xeT], W=[bR[5 + fc % 2]])
                act(sg[fc % 2][:, :], pG[:, :], AF.Silu, R=[bR[3 + fc % 2]], W=[r_sg[fc % 2]])
                tt("dve", hT[:, fc, :], pU[:, :], sg[fc % 2][:, :], ALU.mult, R=[bR[5 + fc % 2], r_sg[fc % 2]], W=[r_hT])
                tick()
            load_next_w(); load_next_w()
            for s in range(4):
                y = ye[s]
                for hf in range(2):
                    pY = banks[7] if hf == 0 else banks[1]
                    rY = bR[7] if hf == 0 else bR[1]
                    for fc in range(8):
                        mm(pY[:, :], hT[:, fc, s * 128:(s + 1) * 128], Wd[:, fc, hf * 512:(hf + 1) * 512], start=(fc == 0), stop=(fc == 7),
                           R=[r_hT, rWd], W=[rY])
                    stt("dve", y[:, hf * 512:(hf + 1) * 512], pY[:, :], ig[:, e_, s, 1:2], g2_bc[:, hf * 512:(hf + 1) * 512], ALU.mult, ALU.mult,
                        R=[rY, r_ig[e_], r_bc2], W=[r_ye[s]])
                    tick()
                if e_ + 1 < NE:
                    xpose(s)
                P.dma("pool", lambda e, s=s, y=y: e.indirect_dma_start(out=acc_d[:, :], out_offset=bass.IndirectOffsetOnAxis(ap=idx_i[:, e_, s:s + 1], axis=0),
                                                                       in_=y[:, :], in_offset=None, compute_op=ALU.add),
                      [r_idx[e_], r_ye[s], r_accd] + (r_acc_e[e_ - 1] if e_ > 0 else []), [r_acc_e[e_][s]])
            load_next_w()

        for _ in compact(0):
            pass
        for _ in compact(1):
            pass
        gather(0)
        for s in range(4):
            xpose(s)
        gather(1)
        for e_ in range(NE):
            bg = compact(e_ + 2) if e_ + 2 < NE else None
            expert(e_, bg)
            if bg is not None:
                for _ in bg:
                    pass
            if e_ + 2 < NE:
                gather(e_ + 2)

        P.barrier()
        cur[0] = MOE_BASE
        fx = [alloc([128, D]) for _ in range(4)]; r_fx = [Res() for _ in range(4)]
        fo = [alloc([128, D]) for _ in range(4)]; r_fo = [Res() for _ in range(4)]
        r_out = [Res() for _ in range(32)]

        def fchain(T):
            i = T % 4
            dma("sp", fx[i][:, :], acc_d[T * 128:(T + 1) * 128, :], R=[r_accd, r_acc_e[NE - 1]], W=[r_fx[i]])
            yield
            yield from ln_stats_g(fx[i], r_fx[i], i, eps=LN_EPS / (ALPHA * ALPHA))
            stt("dve", fo[i][:, :], fx[i][:, :], mv[i][:, 0:1], ln2g_bc[:, :], ALU.subtract, ALU.mult, R=[r_fx[i], r_ln[i], r_bc2], W=[r_fo[i]])
            stt("dve", fo[i][:, :], fo[i][:, :], rstd[i][:, 0:1], ln2b_bc[:, :], ALU.mult, ALU.add, R=[r_fo[i], r_ln[i], r_bc2], W=[r_fo[i]])
            yield
            dma("sp", out_d[T * 128:(T + 1) * 128, :], fo[i][:, :], R=[r_fo[i]], W=[r_out[T]])
        for T0 in range(0, 32, 4):
            run_il([fchain(T) for T in range(T0, T0 + 4)])
        P.barrier()
        P.emit(block)
    return nc


def _na_tables(rel_bias):
    rb = np.asarray(rel_bias, np.float32)
    qrs = [4, 5, 0, 1, 2, 3, 60, 61, 62, 63]
    out = np.full((10, NH, 5, 64, 128), NEG, np.float32)
    qc = np.arange(64)
    c0 = np.clip(qc - 8, 0, 48)
    for v, qr in enumerate(qrs):
        r0 = min(max(qr - 4, 0), 56)
        kt0 = r0 // 2
        nt = 4 if r0 % 2 == 0 else 5
        for ci in range(nt):
            for w2 in range(2):
                kr = 2 * (kt0 + ci) + w2
                if not (r0 <= kr < r0 + 8):
                    continue
                dr = kr - qr + 7
                for kc in range(64):
                    ok = (c0 <= kc) & (kc < c0 + 16)
                    dc = kc - qc + 15
                    vals = rb[:, dr, np.clip(dc, 0, 30)]
                    out[v, :, ci, :, w2 * 64 + kc] = np.where(ok[None, :], vals, NEG)
    return out


def _rope_tables():
    t = np.arange(SEQ)
    row = (t // 64).astype(np.float32)
    col = (t % 64).astype(np.float32)
    inv = (10000.0 ** (-np.arange(0, 16, 2, dtype=np.float32) / 16)).astype(np.float32)
    ang = np.concatenate([row[:, None] * inv, col[:, None] * inv], axis=-1).astype(np.float32)
    cos = np.cos(ang).astype(np.float32).T
    sin = np.sin(ang).astype(np.float32).T
    C = np.concatenate([np.ones((64, SEQ), np.float32), cos, cos], 0)
    S = np.concatenate([np.zeros((64, SEQ), np.float32), -sin, sin], 0)
    return np.ascontiguousarray(C), np.ascontiguousarray(S)


_CACHE = {}


def kernel(x, c, ctx, c_ctx, w_mod, b_mod, w_in, q_norm_g, w_uq, kv_norm_g, w_ukv, na_rel_bias,
           w_proj_mla, w_proj_na, w_out, ln1_g, ln1_b, w_router, w_exp_gate, w_exp_up, w_exp_down,
           ln2_g, ln2_b, _debug=False):
    f = lambda a: np.ascontiguousarray(np.asarray(a, dtype=np.float32))
    x = f(x); c = f(c); ctx = f(ctx); c_ctx = f(c_ctx)
    key = "dbg" if _debug else "nc"
    if key not in _CACHE:
        _CACHE[key] = build_program(debug=_debug)
    nc = _CACHE[key]
    C, S = _rope_tables()
    shared = {
        "w_mod": f(w_mod)[0],
        "bmod_fm": np.ascontiguousarray(f(b_mod)[0].reshape(48, 128).T),
        "bmod_row": f(b_mod)[0].reshape(1, 6 * D),
        "w_in": f(w_in)[0],
        "qg": np.ascontiguousarray(f(q_norm_g)[0].reshape(2, 128).T),
        "kvg": f(kv_norm_g)[0].reshape(128, 1),
        "w_uq": f(w_uq)[0],
        "w_ukv": f(w_ukv)[0],
        "natab": _na_tables(f(na_rel_bias)[0]),
        "w_proj_mla": f(w_proj_mla)[0],
        "w_proj_na": f(w_proj_na)[0],
        "w_out": f(w_out)[0],
        "lnv": np.ascontiguousarray(np.stack([f(ln1_g)[0], f(ln1_b)[0], f(ln2_g)[0], f(ln2_b)[0]], 0)),
        "w_router": f(w_router)[0],
        "w_exp_gate": f(w_exp_gate)[0],
        "w_exp_up": f(w_exp_up)[0],
        "w_exp_down": f(w_exp_down)[0],
        "ropec": C,
        "ropes": S,
    }
    in_maps = []
    for b in range(8):
        cc = np.stack([c[b].reshape(8, 128).T, c_ctx.reshape(8, 128).T], axis=-1)
        m = dict(shared)
        m["x"] = x[b]
        m["ctx"] = ctx[b]
        m["cc"] = np.ascontiguousarray(cc)
        in_maps.append(m)
    res = run_bass_kernel_spmd(nc, in_maps, core_ids=list(range(8)))
    out = np.stack([np.asarray(r["out"], dtype=np.float32) for r in res.results], axis=0)
    if _debug:
        return out, res.results
    return out
```

```python
import numpy as np
from contextlib import ExitStack
import concourse.bass as bass
import concourse.mybir as mybir
from concourse.bass_utils import run_bass_kernel_spmd

F32 = mybir.dt.float32
BF = mybir.dt.bfloat16
I32 = mybir.dt.int32
ALU = mybir.AluOpType
AF = mybir.ActivationFunctionType
AX = mybir.AxisListType

D = 1024
SEQ = 4096
CTX = 256
NKEY = SEQ + CTX
NH = 8
LN_EPS = 1e-5
RMS_EPS = 1e-6
ALPHA = 2.0 ** 0.25
MLA_SCALE = 96.0 ** -0.5
NE = 16
CAP = 512
NEG = -30000.0
KD = 8
NBIS = 30


class Res:
    __slots__ = ("lw", "rd", "name", "psum")

    def __init__(self, name="", psum=False):
        self.lw = None
        self.rd = {}
        self.name = name
        self.psum = psum


class Prog:
    def __init__(self, nc, es):
        self.nc = nc
        self.sems = []
        self.ops = {e: [] for e in ("pe", "act", "dve", "pool", "sp")}
        self.esem = {}
        for e in ("pe", "act", "dve", "pool"):
            self.esem[e] = self._newsem(es, "s_" + e)
        self.cnt = {e: 0 for e in self.esem}
        self.waited = {e: {} for e in self.ops}
        self.dsem = {q: [self._newsem(es, f"d_{q}{i}") for i in range(KD)] for q in ("sp", "pool")}
        self.dcnt = {q: [0] * KD for q in ("sp", "pool")}
        self.dn = {"sp": 0, "pool": 0}

    def _newsem(self, es, name):
        s = es.enter_context(self.nc.semaphore(name))
        self.sems.append(s)
        return len(self.sems) - 1

    def _deps(self, eng, reads, writes, extra=(), is_dma=False):
        deps = {}

        def add(tok):
            if tok is None:
                return
            s, v = tok
            if deps.get(s, 0) < v:
                deps[s] = v
        own = self.esem.get(eng) if not is_dma else None
        for r in reads:
            add(r.lw)
            if r.psum:
                for s, v in r.rd.items():
                    if s != self.esem.get(eng):
                        add((s, v))
        for w in writes:
            if w.lw is not None and w.lw[0] != own:
                add(w.lw)
            for s, v in w.rd.items():
                if s != own:
                    add((s, v))
        for t in extra:
            add(t)
        waits = []
        for s, v in deps.items():
            if eng == "pe" and s == self.esem["pe"]:
                continue
            if self.waited[eng].get(s, 0) < v:
                self.waited[eng][s] = v
                waits.append((s, v))
        return waits

    def _mark(self, tok, reads, writes):
        s, v = tok
        for w in writes:
            w.lw = tok
            w.rd = {}
        for r in reads:
            if r.rd.get(s, 0) < v:
                r.rd[s] = v

    @staticmethod
    def _flat(xs):
        out = []
        for x in xs:
            if isinstance(x, (list, tuple)):
                out.extend(Prog._flat(x))
            else:
                out.append(x)
        return out

    def op(self, eng, fn, reads=(), writes=()):
        reads = self._flat(reads); writes = self._flat(writes)
        waits = self._deps(eng, reads, writes)
        self.cnt[eng] += 1
        tok = (self.esem[eng], self.cnt[eng])
        self.ops[eng].append((waits, fn, self.esem[eng], 1))
        self._mark(tok, reads, writes)
        return tok

    def dma(self, q, fn, reads=(), writes=()):
        reads = self._flat(reads); writes = self._flat(writes)
        i = self.dn[q] % KD
        self.dn[q] += 1
        s = self.dsem[q][i]
        prev = (s, 16 * self.dcnt[q][i]) if self.dcnt[q][i] > 0 else None
        waits = self._deps(q, reads, writes, extra=(prev,), is_dma=True)
        self.dcnt[q][i] += 1
        tok = (s, 16 * self.dcnt[q][i])
        self.ops[q].append((waits, fn, s, 16))
        self._mark(tok, reads, writes)
        return tok

    def barrier(self):
        targets = []
        for e, s in self.esem.items():
            if self.cnt[e] > 0:
                targets.append((s, self.cnt[e]))
        for q in ("sp", "pool"):
            for i in range(KD):
                if self.dcnt[q][i] > 0:
                    targets.append((self.dsem[q][i], 16 * self.dcnt[q][i]))
        for eng in self.ops:
            waits = []
            for s, v in targets:
                if eng == "pe" and s == self.esem["pe"]:
                    continue
                if self.waited[eng].get(s, 0) < v:
                    self.waited[eng][s] = v
                    waits.append((s, v))
            if waits:
                self.ops[eng].append((waits, None, None, 0))

    def emit(self, block):
        sems = self.sems

        def run(eng_name):
            def body(e):
                for waits, fn, s, inc in self.ops[eng_name]:
                    for ws, wv in waits:
                        e.wait_ge(sems[ws], wv)
                    if fn is not None:
                        fn(e).then_inc(sems[s], inc)
            return body
        block.tensor(run("pe"))
        block.scalar(run("act"))
        block.vector(run("dve"))
        block.gpsimd(run("pool"))
        block.sync(run("sp"))


def build_program(debug=False):
    nc = bass.Bass("TRN2", target_bir_lowering=False)

    def din(name, shape, dt=F32):
        return nc.dram_tensor(name, list(shape), dt, kind="ExternalInput").ap()
    x_d = din("x", [SEQ, D])
    ctx_d = din("ctx", [CTX, D])
    cc_d = din("cc", [128, 8, 2])
    wmod_d = din("w_mod", [D, 6 * D])
    bmodfm_d = din("bmod_fm", [128, 48])
    bmodrow_d = din("bmod_row", [1, 6 * D])
    win_d = din("w_in", [D, 4000])
    qg_d = din("qg", [128, 2])
    kvg_d = din("kvg", [128, 1])
    wuq_d = din("w_uq", [256, 768])
    wukv_d = din("w_ukv", [128, 1024])
    natab01_d = din("natab01", [64, NH, 9, 128])
    natabe_d = din("natabe", [8, 64, NH, 4, 128])
    wpm_d = din("w_proj_mla", [512, D])
    wpn_d = din("w_proj_na", [512, D])
    wout_d = din("w_out", [D, D])
    lnv_d = din("lnv", [4, D])
    wr_d = din("w_router", [D, NE])
    wg_d = din("w_exp_gate", [NE, D, D])
    wu_d = din("w_exp_up", [NE, D, D])
    wd_d = din("w_exp_down", [NE, D, D])
    ropec_d = din("ropec", [96, SEQ])
    ropes_d = din("ropes", [96, SEQ])
    out_d = nc.dram_tensor("out", [SEQ, D], F32, kind="ExternalOutput").ap()
    skind = "ExternalOutput" if debug else "Internal"
    u2_d = nc.dram_tensor("u2_d", [SEQ, D], BF, kind=skind).ap()
    acc_d = nc.dram_tensor("acc_d", [SEQ, D], F32, kind=skind).ap()
    yna_d = nc.dram_tensor("yna_d", [512, SEQ], BF, kind=skind).ap()
    g2_d = nc.dram_tensor("g2_d", [128, D], F32, kind="Internal").ap()

    with ExitStack() as es:
        E = es.enter_context
        AW = 52900
        arena = E(nc.sbuf_tensor("arena", [128, AW], F32))
        banks = [E(nc.psum_tensor(f"bank{i}", [128, 512], F32)) for i in range(8)]
        bR = [Res(f"bank{i}", psum=True) for i in range(8)]
        P = Prog(nc, es)
        block = E(nc.Block())

        cur = [0]
        lim = [AW]

        def alloc(shape, dt=F32):
            n = int(np.prod(shape[1:]))
            words = (n * (4 if dt in (F32, I32) else 2) + 3) // 4
            words = (words + 7) // 8 * 8
            off = cur[0]
            cur[0] += words
            assert cur[0] <= lim[0], f"arena overflow {cur[0]} > {lim[0]}"
            v = arena[:, off:off + words]
            if dt != F32:
                v = v.bitcast(dt)
            v = v[:, 0:n]
            if len(shape) == 3:
                v = v.rearrange("p (a b) -> p a b", b=shape[2])
            elif len(shape) == 4:
                v = v.rearrange("p (a b c) -> p a b c", b=shape[2], c=shape[3])
            elif len(shape) == 5:
                v = v.rearrange("p (a b c d) -> p a b c d", b=shape[2], c=shape[3], d=shape[4])
            return v

        def bcast_mid(ap2, n):
            a = ap2.ap
            return bass.AP(ap2.tensor, ap2.offset, [list(a[0]), [0, n], list(a[1])])

        def bcast_part(dap_row, n):
            a = dap_row.ap
            return bass.AP(dap_row.tensor, dap_row.offset, [[0, 128], list(a[-1])])

        def mm(out, lhsT, rhs, start=True, stop=True, R=(), W=()):
            return P.op("pe", lambda e: e.matmul(out, lhsT=lhsT, rhs=rhs, start=start, stop=stop), R, W)

        def tr(out, in_, ident, R=(), W=()):
            return P.op("pe", lambda e: e.transpose(out=out, in_=in_, identity=ident), R, W)

        def act(out, in_, func, R=(), W=(), **kw):
            return P.op("act", lambda e: e.activation(out=out, in_=in_, func=func, **kw), R, W)

        def tt(eng, out, in0, in1, op, R=(), W=()):
            return P.op(eng, lambda e: e.tensor_tensor(out=out, in0=in0, in1=in1, op=op), R, W)

        def ts(eng, out, in0, s1, s2, op0, op1=None, R=(), W=()):
            if op1 is None:
                return P.op(eng, lambda e: e.tensor_scalar(out=out, in0=in0, scalar1=s1, scalar2=None, op0=op0), R, W)
            return P.op(eng, lambda e: e.tensor_scalar(out=out, in0=in0, scalar1=s1, scalar2=s2, op0=op0, op1=op1), R, W)

        def stt(eng, out, in0, scalar, in1, op0, op1, R=(), W=()):
            return P.op(eng, lambda e: e.scalar_tensor_tensor(out=out, in0=in0, scalar=scalar, in1=in1, op0=op0, op1=op1), R, W)

        def cp(eng, out, in_, R=(), W=()):
            if eng == "act":
                return P.op("act", lambda e: e.activation(out=out, in_=in_, func=AF.Copy), R, W)
            return P.op(eng, lambda e: e.tensor_copy(out=out, in_=in_), R, W)

        def recip(out, in_, R=(), W=()):
            return P.op("dve", lambda e: e.reciprocal(out=out, in_=in_), R, W)

        def memset(eng, ap, val, W=()):
            return P.op(eng, lambda e: e.memset(ap, val), (), W)

        def dma(q, out, in_, R=(), W=()):
            return P.dma(q, lambda e: e.dma_start(out=out, in_=in_), R, W)

        ident_bf = alloc([128, 128], BF); r_ident = Res()
        ident_f = alloc([128, 128], F32)
        ones_bf = alloc([128, 128], BF)
        ones_f = alloc([128, 128], F32)
        tri_f = alloc([128, 128], F32)
        neghalf = alloc([128, 8], F32)
        r_const = Res("const")
        modfm = alloc([128, 16, 2], F32); r_modfm = Res()
        sc2p1_bc = alloc([128, D]); sh2_bc = alloc([128, D])
        ln1g_bc = alloc([128, D]); ln1b_bc = alloc([128, D])
        r_bc = Res("bc")
        aff = alloc([128, 32, NE]); r_aff = Res("aff")
        g1_bc = alloc([128, D]); r_g1 = Res()
        st6 = [alloc([128, 12]) for _ in range(10)]
        mv = [alloc([128, 2]) for _ in range(10)]
        rstd = [alloc([128, 1]) for _ in range(10)]
        nbias = [alloc([128, 1]) for _ in range(10)]
        r_ln = [Res(f"ln{i}") for i in range(10)]
        PERSIST = cur[0]

        memset("pool", ident_bf[:, :], 1.0, W=[r_const])
        P.op("pool", lambda e: e.affine_select(out=ident_bf[:, :], in_=ident_bf[:, :], pattern=[[-1, 128]], compare_op=ALU.is_equal,
                                               fill=0.0, base=0, channel_multiplier=1), [r_const], [r_const])
        memset("pool", ident_f[:, :], 1.0, W=[r_const])
        P.op("pool", lambda e: e.affine_select(out=ident_f[:, :], in_=ident_f[:, :], pattern=[[-1, 128]], compare_op=ALU.is_equal,
                                               fill=0.0, base=0, channel_multiplier=1), [r_const], [r_const])
        memset("pool", ones_bf[:, :], 1.0, W=[r_const])
        memset("pool", ones_f[:, :], 1.0, W=[r_const])
        memset("pool", tri_f[:, :], 1.0, W=[r_const])
        P.op("pool", lambda e: e.affine_select(out=tri_f[:, :], in_=tri_f[:, :], pattern=[[1, 128]], compare_op=ALU.is_gt,
                                               fill=0.0, base=0, channel_multiplier=-1), [r_const], [r_const])
        memset("pool", neghalf[:, :], -0.5, W=[r_const])

        cur[0] = PERSIST
        cc_sb = alloc([128, 8, 2]); scc = alloc([128, 8, 2]); r_cc = Res()
        bmodfm = alloc([128, 48]); r_bm = Res()
        wm1 = alloc([128, 8, 2048]); r_wm1 = Res()
        r_g2d = Res()
        dma("sp", cc_sb[:, :, :], cc_d[:, :, :], W=[r_cc])
        dma("sp", bmodfm[:, :], bmodfm_d[:, :], W=[r_bm])
        dma("sp", wm1[:, :, :], wmod_d[:, 0:2048].rearrange("(k p) n -> p k n", p=128), W=[r_wm1])
        for i, t in enumerate((ln1g_bc, ln1b_bc)):
            dma("sp", t[:, :], bcast_part(lnv_d[i:i + 1, :], 128), W=[r_bc])
        act(scc[:, :, :], cc_sb[:, :, :], AF.Silu, R=[r_cc], W=[r_cc])
        pm = banks[0]
        for fc in range(16):
            for k in range(8):
                mm(pm[:, fc * 2:fc * 2 + 2], wm1[:, k, fc * 128:(fc + 1) * 128], scc[:, k, :], start=(k == 0), stop=(k == 7),
                   R=[r_wm1, r_cc], W=[bR[0]])
        pm3 = pm[:, 0:32].rearrange("p (a b) -> p a b", b=2)
        for j in range(2):
            tt("dve", modfm[:, :, j], pm3[:, :, j], bmodfm[:, 0:16], ALU.add, R=[bR[0], r_bm], W=[r_modfm])
        ts("dve", modfm[:, 8:16, :], modfm[:, 8:16, :], 1.0, None, ALU.add, R=[r_modfm], W=[r_modfm])
        def ln_stats(src, rsrc, i, eps=LN_EPS):
            rl = r_ln[i]
            P.op("dve", lambda e: e.bn_stats(out=st6[i][:, 0:6], in_=src[:, 0:512]), [rsrc], [rl])
            P.op("dve", lambda e: e.bn_stats(out=st6[i][:, 6:12], in_=src[:, 512:1024]), [rsrc], [rl])
            P.op("dve", lambda e: e.bn_aggr(out=mv[i][:, :], in_=st6[i][:, :]), [rl], [rl])
            ts("dve", rstd[i][:, :], mv[i][:, 1:2], eps, None, ALU.add, R=[rl], W=[rl])
            tt("pool", rstd[i][:, :], rstd[i][:, :], neghalf[:, 0:1], ALU.pow, R=[rl, r_const], W=[rl])
            stt("dve", nbias[i][:, :], mv[i][:, 0:1], -1.0, rstd[i][:, :], ALU.mult, ALU.mult, R=[rl], W=[rl])

        UTMP = [None, None]

        def run_il(gens):
            gens = list(gens)
            while gens:
                for g in list(gens):
                    try:
                        next(g)
                    except StopIteration:
                        gens.remove(g)

        def run_stag(items):
            items = [[st, g] for st, g in items]
            tick = 0
            while items:
                for it in list(items):
                    if it[0] > tick:
                        continue
                    try:
                        next(it[1])
                    except StopIteration:
                        items.remove(it)
                tick += 1

        def ln_stats_g(src, rsrc, i, eps=LN_EPS):
            rl = r_ln[i]
            P.op("dve", lambda e: e.bn_stats(out=st6[i][:, 0:6], in_=src[:, 0:512]), [rsrc], [rl])
            P.op("dve", lambda e: e.bn_stats(out=st6[i][:, 6:12], in_=src[:, 512:1024]), [rsrc], [rl])
            yield
            P.op("dve", lambda e: e.bn_aggr(out=mv[i][:, :], in_=st6[i][:, :]), [rl], [rl])
            ts("dve", rstd[i][:, :], mv[i][:, 1:2], eps, None, ALU.add, R=[rl], W=[rl])
            yield
            tt("pool", rstd[i][:, :], rstd[i][:, :], neghalf[:, 0:1], ALU.pow, R=[rl, r_const], W=[rl])
            yield
            stt("dve", nbias[i][:, :], mv[i][:, 0:1], -1.0, rstd[i][:, :], ALU.mult, ALU.mult, R=[rl], W=[rl])

        def ln_stats(src, rsrc, i, eps=LN_EPS):
            for _ in ln_stats_g(src, rsrc, i, eps):
                pass

        def ln0_tile(src_d, row0, t, mcol, xts, r_xts, xh, r_xh, uT, r_uT, load, lnset, trbank, skew=0):
            xi = t % 2
            i = lnset
            ptb = banks[trbank].bitcast(BF).rearrange("p (a b) -> p a b", b=128)
            if load:
                dma("sp", xts[xi][:, :], src_d[row0 + t * 128:row0 + (t + 1) * 128, :], W=[r_xts[xi]])
            yield
            yield from ln_stats_g(xts[xi], r_xts[xi], i)
            act(xh[xi][:, :], xts[xi][:, :], AF.Identity, R=[r_xts[xi], r_ln[i]], W=[r_xh[xi]],
                scale=rstd[i][:, 0:1], bias=nbias[i][:, 0:1])
            yield
            for _ in range(skew):
                yield
            for fc in range(8):
                tr(ptb[:, fc, :], xh[xi][:, fc * 128:(fc + 1) * 128], ident_bf[:, :], R=[r_xh[xi], r_const], W=[bR[trbank]])
            pst = modfm.ap[0][0]
            sc_b = bass.AP(modfm.tensor, modfm.offset + 16 + mcol, [[pst, 128], [2, 8], [0, 128]])
            sh_b = bass.AP(modfm.tensor, modfm.offset + mcol, [[pst, 128], [2, 8], [0, 128]])
            tmpu, r_tmpu = UTMP[0], UTMP[1]
            tt("dve", tmpu[:, :, :], ptb[:, :, :], sc_b, ALU.mult, R=[bR[trbank], r_modfm], W=[r_tmpu])
            tt("pool", uT[:, :, t * 128:(t + 1) * 128], tmpu[:, :, :], sh_b, ALU.add, R=[r_tmpu, r_modfm], W=[r_uT[t]])
            yield

        def ln0_block(src_d, row0, ntiles, mcol, xts, r_xts, xh, r_xh, uT, r_uT, keep=False, preloaded=0, trbanks=(0, 0)):
            for t0 in range(0, ntiles, 2):
                run_il([ln0_tile(src_d, row0, t, mcol, xts, r_xts, xh, r_xh, uT, r_uT, t >= preloaded, 4 + t % 2, trbanks[t % 2])
                        for t in range(t0, min(ntiles, t0 + 2))])

        P.barrier()
        cur[0] = PERSIST
        qnT = alloc([128, 2, SEQ], BF); r_qn = [Res() for _ in range(8)]
        kvnT = alloc([128, NKEY], BF); r_kvn = Res()
        krT = alloc([128, NKEY], BF); r_kr = Res()
        LAT_END = cur[0]
        wna = alloc([128, 8, 1536], BF); r_wna = Res()
        wq = alloc([128, 8, 256], BF); wkv = alloc([128, 8, 128], BF)
        wkr = alloc([128, 8, 96], BF); wkrr = alloc([128, 8, 96], BF); r_wl = Res()
        sqb = alloc([128, 2, 512], BF); r_sqb = Res()
        rr_t = alloc([128, 512]); r_rr = Res()
        cqb = alloc([128, 512]); sqb_t = alloc([128, 512]); r_rope = Res()
        tb01 = alloc([128, NH, 9, 128], BF); r_tb01 = Res()
        tbe = alloc([128, NH, 4, 128], BF); r_tbe = Res()
        xts = [alloc([128, D]) for _ in range(2)]; r_xts = [Res(), Res()]
        xh = [alloc([128, D], BF) for _ in range(2)]; r_xh = [Res(), Res()]
        uT = alloc([128, 8, 512], BF); r_uT = [Res() for _ in range(4)]
        UTMP[0] = alloc([128, 8, 128]); UTMP[1] = Res()
        utflat = UTMP[0].rearrange("p a b -> p (a b)")
        t1 = utflat[:, 0:512]; t2 = utflat[:, 512:1024]; r_t1 = UTMP[1]; r_t2 = UTMP[1]
        nqT = [alloc([128, 4, 512], BF) for _ in range(2)]; r_nq = [Res(), Res()]
        NSLOT = 12
        RING = 10
        nkT = alloc([128, 4, NSLOT * 128], BF); r_nk = [Res() for _ in range(NSLOT)]
        nva = alloc([128, NSLOT, NH, 128], BF); r_nv = [Res() for _ in range(NSLOT)]
        pT = [alloc([128, 448], BF) for _ in range(4)]; r_pT = [Res() for _ in range(4)]
        rcn = [alloc([128, 64]) for _ in range(4)]; r_rcn = [Res() for _ in range(4)]
        yblk = [alloc([128, 4, 512], BF) for _ in range(2)]; r_yblk = [Res(), Res()]
        r_yna = [Res() for _ in range(8)]

        dma("pool", wna[:, :, :], win_d[:, 416:1952].rearrange("(k p) n -> p k n", p=128), W=[r_wna])
        memset("pool", nva[:, :, :, 64:128], 1.0, W=r_nv)

        w3 = win_d.rearrange("(k p) n -> p k n", p=128)
        memset("dve", wkr[:, :, 0:64], 0.0, W=[r_wl])
        memset("dve", wkrr[:, :, 0:64], 0.0, W=[r_wl])
        dma("pool", wq[:, :, :], w3[:, :, 0:256], W=[r_wl])
        dma("pool", wkv[:, :, :], w3[:, :, 256:384], W=[r_wl])
        dma("pool", wkr[:, :, 64:96], w3[:, :, 384:416], W=[r_wl])
        dma("pool", wkrr[:, :, 64:80], w3[:, :, 400:416], W=[r_wl])
        dma("pool", wkrr[:, :, 80:96], w3[:, :, 384:400], W=[r_wl])

        def rms_scale(ps_list, rps, nch, N, scale):
            for c2 in range(nch):
                act(sqb[:, c2, 0:N], ps_list[c2], AF.Square, R=[rps[c2]], W=[r_sqb])
            pss = banks[7]
            for c2 in range(nch):
                mm(pss[:, 0:N], ones_bf[:, :], sqb[:, c2, 0:N], start=(c2 == 0), stop=(c2 == nch - 1), R=[r_sqb, r_const], W=[bR[7]])
            ts("dve", rr_t[:, 0:N], pss[:, 0:N], scale, RMS_EPS, ALU.mult, ALU.add, R=[bR[7]], W=[r_rr])
            act(rr_t[:, 0:N], rr_t[:, 0:N], AF.Sqrt, R=[r_rr], W=[r_rr])
            recip(rr_t[:, 0:N], rr_t[:, 0:N], R=[r_rr], W=[r_rr])

        def lat_compute(blk):
            isctx = blk < 0
            N = CTX if isctx else 512
            koff = 0 if isctx else CTX + blk * 512
            pk = banks[1]
            for k in range(8):
                mm(pk[:, 0:N], wkv[:, k, :], uT[:, k, 0:N], start=(k == 0), stop=(k == 7), R=[r_wl, r_uT], W=[bR[1]])
            rms_scale([pk[:, 0:N]], [bR[1]], 1, N, 1.0 / 128)
            tt("dve", kvnT[:, koff:koff + N], pk[:, 0:N], rr_t[:, 0:N], ALU.mult, R=[bR[1], r_rr], W=[r_kvn])
            p1 = banks[2]
            for k in range(8):
                mm(p1[0:96, 0:N], wkr[:, k, :], uT[:, k, 0:N], start=(k == 0), stop=(k == 7), R=[r_wl, r_uT], W=[bR[2]])
            if isctx:
                cp("act", krT[64:96, koff:koff + N], p1[64:96, 0:N], R=[bR[2]], W=[r_kr])
            else:
                p2 = banks[3]
                for k in range(8):
                    mm(p2[0:96, 0:N], wkrr[:, k, :], uT[:, k, 0:N], start=(k == 0), stop=(k == 7), R=[r_wl, r_uT], W=[bR[3]])
                dma("sp", cqb[64:96, :], ropec_d[64:96, blk * 512:(blk + 1) * 512], W=[r_rope])
                dma("sp", sqb_t[64:96, :], ropes_d[64:96, blk * 512:(blk + 1) * 512], W=[r_rope])
                tt("dve", t1[64:96, :], p1[64:96, 0:N], cqb[64:96, :], ALU.mult, R=[bR[2], r_rope], W=[r_t1])
                tt("dve", t2[64:96, :], p2[64:96, 0:N], sqb_t[64:96, :], ALU.mult, R=[bR[3], r_rope], W=[r_t2])
                tt("pool", krT[64:96, koff:koff + N], t1[64:96, :], t2[64:96, :], ALU.add, R=[r_t1, r_t2], W=[r_kr])
                pqs = [banks[4], banks[5]]
                for c2 in range(2):
                    for k in range(8):
                        mm(pqs[c2][:, :], wq[:, k, c2 * 128:(c2 + 1) * 128], uT[:, k, :], start=(k == 0), stop=(k == 7),
                           R=[r_wl, r_uT], W=[bR[4 + c2]])
                rms_scale([pqs[0][:, :], pqs[1][:, :]], [bR[4], bR[5]], 2, 512, 1.0 / 256)
                for c2 in range(2):
                    tt("dve", qnT[:, c2, blk * 512:(blk + 1) * 512], pqs[c2][:, :], rr_t[:, :], ALU.mult, R=[bR[4 + c2], r_rr], W=[r_qn[blk]])

        def na_project(blk):
            isctx = blk < 0
            N = CTX if isctx else 512
            ntl = N // 128
            slots = [RING + t for t in range(ntl)] if isctx else [(blk * 4 + t) % RING for t in range(ntl)]
            par = blk % 2
            for c in range(4):
                if not isctx:
                    pq = banks[1 + c % 2]
                    for k in range(8):
                        mm(pq[:, 0:N], wna[:, k, c * 128:(c + 1) * 128], uT[:, k, 0:N], start=(k == 0), stop=(k == 7),
                           R=[r_wna, r_uT], W=[bR[1 + c % 2]])
                    act(nqT[par][:, c, :], pq[:, 0:N], AF.Identity, R=[bR[1 + c % 2]], W=[r_nq[par]], scale=0.125)
                pk = banks[3 + c % 2]
                for k in range(8):
                    mm(pk[:, 0:N], wna[:, k, 512 + c * 128:512 + (c + 1) * 128], uT[:, k, 0:N], start=(k == 0), stop=(k == 7),
                       R=[r_wna, r_uT], W=[bR[3 + c % 2]])
                for t in range(ntl):
                    s = slots[t]
                    cp("dve", nkT[:, c, s * 128:(s + 1) * 128], pk[:, t * 128:(t + 1) * 128], R=[bR[3 + c % 2]], W=[r_nk[s]])
            for t in range(ntl):
                s = slots[t]
                pv = banks[1 + t % 2]
                for k in range(8):
                    mm(pv[:, :], uT[:, k, t * 128:(t + 1) * 128], wna[:, k, 1024:1536], start=(k == 0), stop=(k == 7),
                       R=[r_wna, r_uT], W=[bR[1 + t % 2]])
                cp("act", nva[:, s, :, 0:64], pv[:, :].rearrange("p (h c) -> p h c", c=64), R=[bR[1 + t % 2]], W=[r_nv[s]])
            lat_compute(blk)

        na_i = [0]
        na_pend = []

        def na_rows(blk, bg=None):
            par = blk % 2
            yb = yblk[par]
            for rr in range(8):
                qr = blk * 8 + rr
                r0 = min(max(qr - 4, 0), 56)
                kt0 = r0 // 2
                nt = 4 if r0 % 2 == 0 else 5
                if 4 <= qr <= 59:
                    tbv = tb01
                    tb0 = 4 * (r0 % 2)
                    r_tb = r_tb01
                else:
                    v = 2 + qr if qr < 4 else 6 + (qr - 60)
                    for half in range(2):
                        dma("pool", tbe[half * 64:(half + 1) * 64, :, :, :], natabe_d[v - 2], W=[r_tbe])
                    tbv = tbe
                    tb0 = 0
                    r_tb = r_tbe
                qoff = rr * 64
                tiles = [(kt0 + ci) % RING for ci in range(nt)] + [RING, RING + 1]
                ncol = len(tiles) * 64
                for hp in range(4):
                    k2 = na_i[0] % 2
                    na_i[0] += 1
                    psb = (5, 6) if k2 == 0 else (1, 2)
                    pob = (7, 0) if k2 == 0 else (3, 4)
                    c = hp
                    for ci, s in enumerate(tiles):
                        loc = ci < nt
                        for hh in range(2):
                            pb = hh * 64
                            mm(banks[psb[hh]][:, ci * 64:(ci + 1) * 64], nkT[pb:pb + 64, c, s * 128:(s + 1) * 128],
                               nqT[par][pb:pb + 64, c, qoff:qoff + 64], start=True, stop=not loc,
                               R=[r_nk[s], r_nq[par]], W=[bR[psb[hh]]])
                        if loc:
                            for hh in range(2):
                                pb = hh * 64
                                mm(banks[psb[hh]][:, ci * 64:(ci + 1) * 64], tbv[pb:pb + 64, 2 * hp + hh, tb0 + ci, :], ident_bf[pb:pb + 64, pb:pb + 64],
                                   start=False, stop=True, R=[r_tb, r_const], W=[bR[psb[hh]]])
                    if na_pend:
                        na_pend.pop()()

                    def fin(k2=k2, hp=hp, c=c, tiles=tiles, ncol=ncol, qoff=qoff, yb=yb, par=par, psb=psb, pob=pob):
                        for hh in range(2):
                            i = 2 * k2 + hh
                            h = 2 * hp + hh
                            pb = hh * 64
                            ps = banks[psb[hh]]
                            act(pT[i][:, 0:ncol], ps[:, 0:ncol], AF.Exp, R=[bR[psb[hh]]], W=[r_pT[i]])
                        for hh in range(2):
                            i = 2 * k2 + hh
                            h = 2 * hp + hh
                            pb = hh * 64
                            po = banks[pob[hh]]
                            rpo = bR[pob[hh]]
                            for ci, s in enumerate(tiles):
                                mm(po[:, 0:64], nva[:, s, h, :], pT[i][:, ci * 64:(ci + 1) * 64], start=(ci == 0), stop=(ci == len(tiles) - 1),
                                   R=[r_nv[s], r_pT[i]], W=[rpo])
                            recip(rcn[i][0:64, :], po[64:128, 0:64], R=[rpo], W=[r_rcn[i]])
                            tt("dve", yb[pb:pb + 64, c, qoff:qoff + 64], po[0:64, 0:64], rcn[i][0:64, :], ALU.mult, R=[rpo, r_rcn[i]], W=[r_yblk[par]])
                    na_pend.append(fin)
                    if bg is not None:
                        next(bg, None)
                        next(bg, None)
            if na_pend:
                na_pend.pop()()

        def na_store(blk):
            dma("sp", yna_d.rearrange("(c p) t -> p c t", p=128)[:, :, blk * 512:(blk + 1) * 512], yblk[blk % 2][:, :, :],
                R=[r_yblk[blk % 2]], W=[r_yna[blk]])

        def ln0_bg(blk):
            for t0 in (0, 2):
                gens = [ln0_tile(x_d, blk * 512, t, 0, xts, r_xts, xh, r_xh, uT, r_uT, True, 4 + t % 2, 0, skew=4) for t in (t0, t0 + 1)]
                while gens:
                    for g in list(gens):
                        try:
                            next(g)
                        except StopIteration:
                            gens.remove(g)
                        yield

        ln0_block(ctx_d, 0, 2, 1, xts, r_xts, xh, r_xh, uT, r_uT)
        na_project(-1)
        ln0_block(x_d, 0, 4, 0, xts, r_xts, xh, r_xh, uT, r_uT)
        for half in range(2):
            dma("pool", tb01[half * 64:(half + 1) * 64, :, :, :], natab01_d[:, :, :, :], W=[r_tb01])
        for blk in range(8):
            na_project(blk)
            bg = ln0_bg(blk + 1) if blk + 1 < 8 else None
            if blk >= 2:
                na_store(blk - 2)
            if blk >= 1:
                na_rows(blk - 1, bg)
            if bg is not None:
                for _ in bg:
                    pass
        na_store(6)
        na_rows(7)
        na_store(7)

        P.barrier()
        cur[0] = AW - 8192
        y_mlaT = alloc([128, 4, SEQ], BF); r_ymla = [Res() for _ in range(8)]
        lim[0] = AW - 8192
        cur[0] = LAT_END
        wuq = alloc([128, 2, 768], BF); wuqr = alloc([128, 2, NH, 96], BF); wukv = alloc([128, 1024], BF); r_wu = Res()
        stg = alloc([128, 2, 768]); r_stg = Res()
        qg_sb = alloc([128, 2]); kvg_sb = alloc([128, 1]); r_g = Res()
        kh = [alloc([128, NKEY], BF) for _ in range(2)]; r_kh = [Res(), Res()]
        vm = [alloc([128, 34, 128], BF) for _ in range(2)]; r_vm = [Res(), Res()]
        qh = [alloc([128, 512], BF) for _ in range(2)]; r_qh = [Res(), Res()]
        cq2 = [alloc([128, 512]) for _ in range(2)]; sq2 = [alloc([128, 512]) for _ in range(2)]; r_rp = [Res(), Res()]
        t1 = alloc([128, 512]); t2 = alloc([128, 512]); r_t1 = Res(); r_t2 = Res()
        pTm = [alloc([128, 512], BF) for _ in range(3)]; r_pTm = [Res() for _ in range(3)]
        rcm = [alloc([128, 512]) for _ in range(2)]; r_rcm = [Res(), Res()]

        dma("sp", qg_sb[:, :], qg_d[:, :], W=[r_g])
        dma("sp", kvg_sb[:, :], kvg_d[:, :], W=[r_g])
        dma("sp", stg[:, :, :], wuq_d.rearrange("(k p) n -> p k n", p=128), W=[r_stg])
        for c2 in range(2):
            ts("dve", wuq[:, c2, :], stg[:, c2, :], qg_sb[:, c2:c2 + 1], None, ALU.mult, R=[r_stg, r_g], W=[r_wu])
        memset("pool", wuqr[:, :, :, 0:64], 0.0, W=[r_wu])
        wuq4 = wuq.rearrange("p k (h c) -> p k h c", c=96)
        for c2 in range(2):
            cp("dve", wuqr[:, c2, :, 64:80], wuq4[:, c2, :, 80:96], R=[r_wu], W=[r_wu])
            cp("dve", wuqr[:, c2, :, 80:96], wuq4[:, c2, :, 64:80], R=[r_wu], W=[r_wu])
        stg_kv = stg.rearrange("p a b -> p (a b)")[:, 0:1024]; r_stgkv = r_stg
        dma("sp", stg_kv[:, :], wukv_d[:, :], W=[r_stgkv])
        ts("dve", wukv[:, :], stg_kv[:, :], kvg_sb[:, 0:1], None, ALU.mult, R=[r_stgkv, r_g], W=[r_wu])
        for i in range(2):
            memset("pool", vm[i][:, :, 64:128], 1.0, W=[r_vm[i]])

        def build_kv(h):
            hp = h % 2
            for j in range(9):
                n = 512 if j < 8 else 256
                pk = banks[6]
                mm(pk[0:64, 0:n], wukv[:, h * 128:h * 128 + 64], kvnT[:, j * 512:j * 512 + n], R=[r_wu, r_kvn], W=[bR[6]])
                cp("dve", kh[hp][0:64, j * 512:j * 512 + n], pk[0:64, 0:n], R=[bR[6]], W=[r_kh[hp]])
                yield
            cp("pool", kh[hp][64:96, :], krT[64:96, :], R=[r_kr], W=[r_kh[hp]])
            for g0 in range(0, 34, 8):
                gn = min(8, 34 - g0)
                pv = banks[7]
                for kt in range(g0, g0 + gn):
                    mm(pv[:, (kt - g0) * 64:(kt - g0 + 1) * 64], kvnT[:, kt * 128:(kt + 1) * 128], wukv[:, h * 128 + 64:h * 128 + 128],
                       R=[r_wu, r_kvn], W=[bR[7]])
                cp("dve", vm[hp][:, g0:g0 + gn, 0:64], pv[:, 0:gn * 64].rearrange("p (a b) -> p a b", b=64), R=[bR[7]], W=[r_vm[hp]])
                yield

        def build_q(it):
            h, qb = it // 8, it % 8
            ip = it % 2
            dma("sp", cq2[ip][0:96, :], ropec_d[:, qb * 512:(qb + 1) * 512], W=[r_rp[ip]])
            dma("sp", sq2[ip][0:96, :], ropes_d[:, qb * 512:(qb + 1) * 512], W=[r_rp[ip]])
            yield
            p1 = banks[5]
            rb1 = bR[5]
            for c2 in range(2):
                mm(p1[0:96, :], wuq[:, c2, h * 96:(h + 1) * 96], qnT[:, c2, qb * 512:(qb + 1) * 512], start=(c2 == 0), stop=(c2 == 1),
                   R=[r_wu, r_qn[qb]], W=[rb1])
            tt("dve", t1[0:96, :], p1[0:96, :], cq2[ip][0:96, :], ALU.mult, R=[rb1, r_rp[ip]], W=[r_t1])
            yield
            for c2 in range(2):
                mm(p1[0:96, :], wuqr[:, c2, h, :], qnT[:, c2, qb * 512:(qb + 1) * 512], start=(c2 == 0), stop=(c2 == 1),
                   R=[r_wu, r_qn[qb]], W=[rb1])
            tt("dve", t2[0:96, :], p1[0:96, :], sq2[ip][0:96, :], ALU.mult, R=[rb1, r_rp[ip]], W=[r_t2])
            yield
            tt("pool", qh[ip][0:96, :], t1[0:96, :], t2[0:96, :], ALU.add, R=[r_t1, r_t2], W=[r_qh[ip]])

        def attention(it, bgs):
            h, qb = it // 8, it % 8
            ip = it % 2
            hp = h % 2
            po = banks[3 + ip]

            def s_issue(kc):
                si = kc % 3
                mm(banks[si][:, :], kh[hp][0:96, kc * 128:(kc + 1) * 128], qh[ip][0:96, :], R=[r_kh[hp], r_qh[ip]], W=[bR[si]])
            for kc in range(3):
                s_issue(kc)
            for kc in range(34):
                si = kc % 3
                act(pTm[si][:, :], banks[si][:, :], AF.Exp, R=[bR[si]], W=[r_pTm[si]], scale=MLA_SCALE)
                mm(po[:, :], vm[hp][:, kc, :], pTm[si][:, :], start=(kc == 0), stop=(kc == 33), R=[r_vm[hp], r_pTm[si]], W=[bR[3 + ip]])
                if kc + 3 < 34:
                    s_issue(kc + 3)
                if kc % 4 == 1:
                    for g in bgs:
                        next(g, None)
                if kc == 17:
                    next(modgen, None)
            recip(rcm[ip][0:64, :], po[64:128, :], R=[bR[3 + ip]], W=[r_rcm[ip]])
            tt("dve", y_mlaT[hp * 64:hp * 64 + 64, h // 2, qb * 512:(qb + 1) * 512], po[0:64, :], rcm[ip][0:64, :], ALU.mult,
               R=[bR[3 + ip], r_rcm[ip]], W=[r_ymla[qb]])

        cc2 = alloc([128, 8, 2]); scc2 = alloc([128, 8, 2]); r_cc2 = Res()
        screp = alloc([128, 8, 128]); r_screp = Res()
        wmc = [alloc([128, 8, 256]) for _ in range(2)]; r_wmc = [Res(), Res()]
        bmb = [alloc([128, 256]) for _ in range(2)]; r_bmb = [Res(), Res()]
        g2_t = alloc([128, D]); r_g2 = Res()

        def modbc_gen():
            dma("sp", cc2[:, :, :], cc_d[:, :, :], W=[r_cc2])
            yield
            act(scc2[:, :, :], cc2[:, :, :], AF.Silu, R=[r_cc2], W=[r_cc2])
            for k in range(8):
                ts("dve", screp[:, k, :], ones_f[:, :], scc2[:, k, 0:1], None, ALU.mult, R=[r_cc2, r_const], W=[r_screp])
            yield
            dsts = [g1_bc, sh2_bc, sc2p1_bc, g2_t]
            rds = [r_g1, r_bc, r_bc, r_g2]
            for jj in range(16):
                f0 = 2048 + jj * 256
                reg = (jj * 256) // 1024
                c0 = (jj * 256) % 1024
                wb = wmc[jj % 2]; bb = bmb[jj % 2]
                dma("sp", wb[:, :, :], wmod_d[:, f0:f0 + 256].rearrange("(k p) n -> p k n", p=128), W=[r_wmc[jj % 2]])
                dma("sp", bb[:, :], bcast_part(bmodrow_d[0:1, f0:f0 + 256], 128), W=[r_bmb[jj % 2]])
                yield
                pb = banks[5]
                for k in range(8):
                    mm(pb[:, 0:256], screp[:, k, :], wb[:, k, :], start=(k == 0), stop=(k == 7), R=[r_screp, r_wmc[jj % 2]], W=[bR[5]])
                dst = dsts[reg][:, c0:c0 + 256]
                tt("dve", dst, pb[:, 0:256], bb[:, :], ALU.add, R=[bR[5], r_bmb[jj % 2]], W=[rds[reg]])
                if reg == 2:
                    ts("dve", dst, dst, 1.0, None, ALU.add, R=[rds[reg]], W=[rds[reg]])
                elif reg == 3:
                    ts("dve", dst, dst, 1.0 / ALPHA, None, ALU.mult, R=[rds[reg]], W=[rds[reg]])
                yield
            dma("sp", g2_d[:, :], g2_t[:, :], R=[r_g2], W=[r_g2d])

        modgen = modbc_gen()
        for _ in build_kv(0):
            pass
        for _ in build_q(0):
            pass
        kvgen = None
        for it in range(64):
            if it % 8 == 0:
                if kvgen is not None:
                    for _ in kvgen:
                        pass
                kvgen = build_kv(it // 8 + 1) if it // 8 + 1 < NH else None
            qgen = build_q(it + 1) if it + 1 < 64 else None
            attention(it, [g for g in (qgen, kvgen) if g is not None])
            if qgen is not None:
                for _ in qgen:
                    pass
        for _ in modgen:
            pass

        P.barrier()
        cur[0] = PERSIST
        wg_in = alloc([128, 8, 2048], BF); r_wgin = Res()
        wpm = alloc([128, 4, D], BF); wpn = alloc([128, 4, D], BF); r_wp = Res()
        wout = alloc([128, 8, D], BF); r_wout = Res()
        wr_sb = alloc([128, 8, NE]); r_wr = Res()
        xts = [alloc([128, D]) for _ in range(2)]; r_xts = [Res() for _ in range(2)]
        xh = [alloc([128, D], BF) for _ in range(2)]; r_xh = [Res(), Res()]
        uTs = [alloc([128, 8, 512], BF) for _ in range(2)]; r_uTs = [[Res() for _ in range(4)] for _ in range(2)]
        mTs = [alloc([128, 8, 512], BF) for _ in range(2)]; r_mTs = [Res(), Res()]
        ynab = alloc([128, 4, 512], BF); r_ynab = Res()
        sig_all = g1_bc
        sig = [sig_all[:, 0:512], sig_all[:, 512:1024]]; r_sig = [r_g1, r_g1]
        tAB = alloc([128, 1024]); tA = tAB[:, 0:512]; tB = tAB[:, 512:1024]; r_tA = Res(); r_tB = r_tA
        UTMP[0] = alloc([128, 8, 128]); UTMP[1] = Res()
        Xs = [alloc([128, D]) for _ in range(4)]; r_Xs = [Res() for _ in range(4)]
        u2bs = [alloc([128, D], BF) for _ in range(2)]; r_u2bs = [Res(), Res()]
        u2Ts = [alloc([128, 8, 128]) for _ in range(2)]; r_u2Ts = [Res(), Res()]
        sms = [alloc([128, 8]) for _ in range(2)]; r_sms = [Res(), Res()]
        exs = [alloc([128, NE]) for _ in range(2)]; r_exs = [Res(), Res()]
        r_u2d_t = [Res() for _ in range(32)]; r_accd_t = [Res() for _ in range(32)]

        dma("pool", wg_in[:, :, :], w3[:, :, 1952:4000], W=[r_wgin])
        dma("pool", wpm[:, :, :], wpm_d.rearrange("(k p) n -> p k n", p=128), W=[r_wp])
        dma("pool", wpn[:, :, :], wpn_d.rearrange("(k p) n -> p k n", p=128), W=[r_wp])
        dma("sp", wr_sb[:, :, :], wr_d.rearrange("(k p) n -> p k n", p=128), W=[r_wr])
        wo3 = wout_d.rearrange("(k p) n -> p k n", p=128)
        for k in range(8):
            dma("sp", xts[k % 2][:, :], wo3[:, k, :], W=[r_xts[k % 2]])
            tt("dve" if k % 2 == 0 else "pool", wout[:, k, :], xts[k % 2][:, :], g1_bc[:, :], ALU.mult, R=[r_xts[k % 2], r_g1], W=[r_wout])

        def dcgen(blk):
            uT = uTs[blk % 2]; r_uT = r_uTs[blk % 2]; mT = mTs[blk % 2]; r_mT = r_mTs[blk % 2]
            dma("sp", ynab[:, :, :], yna_d.rearrange("(c p) t -> p c t", p=128)[:, :, blk * 512:(blk + 1) * 512], R=[r_yna[blk]], W=[r_ynab])
            yield
            for dc in range(8):
                for gi in range(2):
                    pg = banks[1 + gi]
                    for k in range(8):
                        mm(pg[:, :], wg_in[:, k, gi * 1024 + dc * 128:gi * 1024 + (dc + 1) * 128], uT[:, k, :], start=(k == 0), stop=(k == 7),
                           R=[r_wgin, r_uT], W=[bR[1 + gi]])
                    act(sig[gi][:, :], pg[:, :], AF.Sigmoid, R=[bR[1 + gi]], W=[r_sig[gi]])
                    yield
                pA = banks[3]; pB = banks[4]
                for c in range(4):
                    mm(pA[:, :], wpm[:, c, dc * 128:(dc + 1) * 128], y_mlaT[:, c, blk * 512:(blk + 1) * 512], start=(c == 0), stop=(c == 3),
                       R=[r_wp, r_ymla[blk]], W=[bR[3]])
                for c in range(4):
                    mm(pB[:, :], wpn[:, c, dc * 128:(dc + 1) * 128], ynab[:, c, :], start=(c == 0), stop=(c == 3), R=[r_wp, r_ynab], W=[bR[4]])
                tt("dve", tA[:, :], pA[:, :], sig[0][:, :], ALU.mult, R=[bR[3], r_sig[0]], W=[r_tA])
                tt("dve", tB[:, :], pB[:, :], sig[1][:, :], ALU.mult, R=[bR[4], r_sig[1]], W=[r_tB])
                tt("pool", mT[:, dc, :], tA[:, :], tB[:, :], ALU.add, R=[r_tA, r_tB], W=[r_mT])
                yield

        def tchain(blk, t):
            T = blk * 4 + t
            p = t % 2
            mT = mTs[blk % 2]; r_mT = r_mTs[blk % 2]
            X = Xs[t]; rX = r_Xs[t]
            u2b = u2bs[p]; r_u2b = r_u2bs[p]; u2T = u2Ts[p]; r_u2T = r_u2Ts[p]; sm = sms[p]; r_sm = r_sms[p]; ex = exs[p]; r_ex = r_exs[p]
            l0 = 2 * t; l1 = 2 * t + 1
            dma("sp", X[:, :], x_d[T * 128:(T + 1) * 128, :], W=[rX])
            yield
            for hf in range(2):
                bi = 5 + hf
                pm_ = banks[bi]
                for k in range(8):
                    mm(pm_[:, :], mT[:, k, t * 128:(t + 1) * 128], wout[:, k, hf * 512:(hf + 1) * 512], start=(k == 0), stop=(k == 7),
                       R=[r_mT, r_wout], W=[bR[bi]])
                stt("dve", X[:, hf * 512:(hf + 1) * 512], X[:, hf * 512:(hf + 1) * 512], ALPHA, pm_[:, :], ALU.mult, ALU.add,
                    R=[rX, bR[bi]], W=[rX])
                yield
            yield from ln_stats_g(X, rX, l0)
            stt("dve", X[:, :], X[:, :], mv[l0][:, 0:1], ln1g_bc[:, :], ALU.subtract, ALU.mult, R=[rX, r_ln[l0], r_bc], W=[rX])
            stt("dve", X[:, :], X[:, :], rstd[l0][:, 0:1], ln1b_bc[:, :], ALU.mult, ALU.add, R=[rX, r_ln[l0], r_bc], W=[rX])
            yield
            dma("sp", acc_d[T * 128:(T + 1) * 128, :], X[:, :], R=[rX], W=[r_accd_t[T]])
            yield from ln_stats_g(X, rX, l1)
            stt("dve", X[:, :], X[:, :], mv[l1][:, 0:1], sc2p1_bc[:, :], ALU.subtract, ALU.mult, R=[rX, r_ln[l1], r_bc], W=[rX])
            stt("dve", X[:, :], X[:, :], rstd[l1][:, 0:1], sh2_bc[:, :], ALU.mult, ALU.add, R=[rX, r_ln[l1], r_bc], W=[rX])
            yield
            cp("act", u2b[:, :], X[:, :], R=[rX], W=[r_u2b])
            dma("sp", u2_d[T * 128:(T + 1) * 128, :], u2b[:, :], R=[r_u2b], W=[r_u2d_t[T]])
            pt_ = banks[7]
            for hf in range(2):
                for q4 in range(4):
                    fc = hf * 4 + q4
                    tr(pt_[:, q4 * 128:(q4 + 1) * 128], X[:, fc * 128:(fc + 1) * 128], ident_f[:, :], R=[rX, r_const], W=[bR[7]])
                cp("act" if hf == 0 else "dve", u2T[:, hf * 4:(hf + 1) * 4, :], pt_[:, :].rearrange("p (a b) -> p a b", b=128), R=[bR[7]], W=[r_u2T])
                yield
            pl = banks[7]
            for k in range(8):
                mm(pl[:, 0:NE], u2T[:, k, :], wr_sb[:, k, :], start=(k == 0), stop=(k == 7), R=[r_u2T, r_wr], W=[bR[7]])
            P.op("dve", lambda e, pl=pl, sm=sm: e.reduce_max(out=sm[:, 0:1], in_=pl[:, 0:NE], axis=AX.X), [bR[7]], [r_sm])
            ts("dve", sm[:, 1:2], sm[:, 0:1], -1.0, None, ALU.mult, R=[r_sm], W=[r_sm])
            act(ex[:, :], pl[:, 0:NE], AF.Exp, R=[bR[7], r_sm], W=[r_ex, r_sm], bias=sm[:, 1:2], accum_out=sm[:, 2:3])
            recip(sm[:, 3:4], sm[:, 2:3], R=[r_sm], W=[r_sm])
            ts("dve", aff[:, T, :], ex[:, :], sm[:, 3:4], None, ALU.mult, R=[r_ex, r_sm], W=[r_aff])

        def ln0_items(blk, first_tick):
            uT = uTs[blk % 2]; r_uT = r_uTs[blk % 2]
            return [(first_tick + 7 * (t // 2), ln0_tile(x_d, blk * 512, t, 0, xts, r_xts, xh, r_xh, uT, r_uT, True, 8 + t % 2, 0)) for t in range(4)]

        ln0_block(x_d, 0, 4, 0, xts, r_xts, xh, r_xh, uTs[0], r_uTs[0])
        run_stag([(0, dcgen(0))] + ln0_items(1, 1))
        STG = 4
        for blk in range(8):
            items = [(0, tchain(blk, 0)), (0, tchain(blk, 1)), (STG, tchain(blk, 2)), (STG, tchain(blk, 3))]
            if blk + 1 < 8:
                items.append((0, dcgen(blk + 1)))
            if blk + 2 < 8:
                items += ln0_items(blk + 2, 1)
            run_stag(items)

        P.barrier()
        lim[0] = AW
        r_u2d = Res("u2d"); r_accd = Res("accd")
        cur[0] = PERSIST
        g2_bc = alloc([128, D]); ln2g_bc = alloc([128, D]); ln2b_bc = alloc([128, D]); r_bc2 = Res()
        dma("sp", g2_bc[:, :], g2_d[:, :], R=[r_g2d], W=[r_bc2])
        dma("sp", ln2g_bc[:, :], bcast_part(lnv_d[2:3, :], 128), W=[r_bc2])
        dma("sp", ln2b_bc[:, :], bcast_part(lnv_d[3:4, :], 128), W=[r_bc2])
        MOE_BASE = cur[0]
        wring = [alloc([128, 8, D], BF) for _ in range(4)]; r_wring = [Res() for _ in range(4)]
        lo = alloc([128, NE]); hi = alloc([128, NE]); mid = alloc([128, NE]); r_th = Res()
        cmpm = alloc([128, 32, NE]); r_cmp = Res()
        cntp = alloc([128, NE]); r_cntp = Res()
        ge = alloc([128, NE]); dlo = alloc([128, NE]); dhi = alloc([128, NE]); r_ge = Res()
        base = alloc([128, NE]); r_base = Res()
        cs = alloc([128, NE, 32]); posm = alloc([128, NE, 32]); r_pos = Res()
        rcb = alloc([128, NE, 32, 6], BF); r_rc2 = Res()
        gsp = alloc([128, NE, 32]); gsb = alloc([128, NE, 32], BF); gsf = alloc([128, NE, 32]); r_gs = Res()
        tj_i = alloc([128, 32], I32); tp_i = alloc([128, 32], I32)
        tokid_i = alloc([128, 32], I32); tokid = alloc([128, 32]); iota_i = alloc([128, 512], I32); iota_s = alloc([128, 512]); r_io = Res()
        oh = [alloc([128, 512], BF) for _ in range(4)]; r_oh = [Res() for _ in range(4)]
        cmp_sb = alloc([128, 512]); r_cmps = Res()
        ig5 = alloc([128, 4, 6]); r_ig5 = Res()
        ig = alloc([128, NE, 4, 2]); r_ig = [Res() for _ in range(NE)]
        idx_i = alloc([128, NE, 4], I32); r_idx = [Res() for _ in range(NE)]
        xe = alloc([128, 4, D], BF); r_xe = Res()
        xeT = alloc([128, 8, 512], BF); r_xeT = Res()
        hT = alloc([128, 8, 512], BF); r_hT = Res()
        sg = [alloc([128, 512]) for _ in range(2)]; r_sg = [Res(), Res()]
        ye = [alloc([128, D]) for _ in range(4)]; r_ye = [Res() for _ in range(4)]
        r_acc_e = [[Res() for _ in range(4)] for _ in range(NE)]

        wsrc = []
        for e_ in range(NE):
            wsrc += [wg_d[e_], wu_d[e_], wd_d[e_]]
        wload = [0]

        def load_next_w():
            i = wload[0]
            if i >= len(wsrc):
                return
            wload[0] += 1
            dma("pool", wring[i % 4][:, :, :], wsrc[i].rearrange("(k p) n -> p k n", p=128), W=[r_wring[i % 4]])
        for _ in range(4):
            load_next_w()

        P.op("pool", lambda e: e.iota(tj_i[:, :], pattern=[[1, 32]], base=0, channel_multiplier=0), (), [r_io])
        P.op("pool", lambda e: e.iota(tp_i[:, :], pattern=[[0, 32]], base=0, channel_multiplier=1), (), [r_io])
        P.op("pool", lambda e: e.iota(iota_i[:, :], pattern=[[1, 512]], base=0, channel_multiplier=0), (), [r_io])
        cp("dve", iota_s[:, :], iota_i[:, :], R=[r_io], W=[r_io])
        memset("dve", lo[:, :], 0.0, W=[r_th])
        memset("dve", hi[:, :], 1.0, W=[r_th])
        aff_ej = aff.rearrange("p j e -> p e j")

        def count_ge(thr):
            tt("dve", cmpm[:, :, :], aff[:, :, :], bcast_mid(thr[:, :], 32), ALU.is_ge, R=[r_aff, r_th], W=[r_cmp])
            P.op("dve", lambda e: e.tensor_reduce(out=cntp[:, :], in_=cmpm.rearrange("p j e -> p e j"), axis=AX.X, op=ALU.add), [r_cmp], [r_cntp])
        for it in range(NBIS):
            hw = 0.5 ** (it + 1)
            ts("dve", mid[:, :], lo[:, :], hw, None, ALU.add, R=[r_th], W=[r_th])
            count_ge(mid)
            pc = banks[it % 2]
            mm(pc[:, 0:NE], ones_f[:, :], cntp[:, :], R=[r_cntp, r_const], W=[bR[it % 2]])
            ts("dve", ge[:, :], pc[:, 0:NE], CAP - 0.5, hw, ALU.is_gt, ALU.mult, R=[bR[it % 2]], W=[r_ge])
            tt("dve", lo[:, :], lo[:, :], ge[:, :], ALU.add, R=[r_ge, r_th], W=[r_th])
        count_ge(lo)
        pbse = banks[2]
        mm(pbse[:, 0:NE], tri_f[:, :], cntp[:, :], R=[r_cntp, r_const], W=[bR[2]])
        cp("dve", base[:, :], pbse[:, 0:NE], R=[bR[2]], W=[r_base])
        m_ej = cmpm.rearrange("p j e -> p e j")
        for e_ in range(NE):
            P.op("dve", lambda e, e_=e_: e.tensor_tensor_scan(out=cs[:, e_, :], data0=ones_f[:, 0:32], data1=m_ej[:, e_, :], initial=0.0,
                                                              op0=ALU.mult, op1=ALU.add), [r_cmp, r_const], [r_pos])
            stt("dve", posm[:, e_, :], cs[:, e_, :], base[:, e_:e_ + 1], m_ej[:, e_, :], ALU.add, ALU.subtract, R=[r_pos, r_base, r_cmp], W=[r_pos])
            stt("dve", posm[:, e_, :], posm[:, e_, :], 1.0, m_ej[:, e_, :], ALU.add, ALU.mult, R=[r_pos, r_cmp], W=[r_pos])
            ts("dve", posm[:, e_, :], posm[:, e_, :], -1.0, None, ALU.add, R=[r_pos], W=[r_pos])
            cp("pool", rcb[:, e_, :, 0], tj_i[:, :], R=[r_io], W=[r_rc2])
            cp("pool", rcb[:, e_, :, 1], tp_i[:, :], R=[r_io], W=[r_rc2])
            tt("pool", gsp[:, e_, :], aff_ej[:, e_, :], m_ej[:, e_, :], ALU.mult, R=[r_aff, r_cmp], W=[r_gs])
        memset("pool", rcb[:, :, :, 5], 0.0, W=[r_rc2])
        for part in range(3):
            cp("dve", gsb[:, :, :], gsp[:, :, :], R=[r_gs], W=[r_gs])
            cp("dve", rcb[:, :, :, 2 + part], gsb[:, :, :], R=[r_gs], W=[r_rc2])
            if part < 2:
                cp("dve", gsf[:, :, :], gsb[:, :, :], R=[r_gs], W=[r_gs])
                tt("dve", gsp[:, :, :], gsp[:, :, :], gsf[:, :, :], ALU.subtract, R=[r_gs], W=[r_gs])

        def compact(e_):
            pc = banks[0]
            pend = []
            for j in range(32):
                o = oh[j % 4]
                ts("dve", o[:, :], iota_s[:, :], posm[:, e_, j:j + 1], None, ALU.is_equal, R=[r_io, r_pos], W=[r_oh[j % 4]])
                pend.append(j)
                if j % 2 == 1:
                    while len(pend) > 2:
                        jj = pend.pop(0)
                        mm(pc[0:6, :], rcb[:, e_, jj, :], oh[jj % 4][:, :], start=(jj == 0), stop=False, R=[r_rc2, r_oh[jj % 4]], W=[bR[0]])
                    yield
            while pend:
                jj = pend.pop(0)
                mm(pc[0:6, :], rcb[:, e_, jj, :], oh[jj % 4][:, :], start=(jj == 0), stop=(jj == 31), R=[r_rc2, r_oh[jj % 4]], W=[bR[0]])
            cp("dve", cmp_sb[0:6, :], pc[0:6, :], R=[bR[0]], W=[r_cmps])
            yield
            pt_ = banks[0]
            for s in range(4):
                tr(pt_[:, s * 6:(s + 1) * 6], cmp_sb[0:6, s * 128:(s + 1) * 128], ident_f[0:6, 0:6], R=[r_cmps, r_const], W=[bR[0]])
            cp("dve", ig5[:, :, :], pt_[:, 0:24].rearrange("p (a b) -> p a b", b=6), R=[bR[0]], W=[r_ig5])
            stt("dve", ig[:, e_, :, 0], ig5[:, :, 0], 128.0, ig5[:, :, 1], ALU.mult, ALU.add, R=[r_ig5], W=[r_ig[e_]])
            tt("dve", ig[:, e_, :, 1], ig5[:, :, 2], ig5[:, :, 3], ALU.add, R=[r_ig5], W=[r_ig[e_]])
            tt("dve", ig[:, e_, :, 1], ig[:, e_, :, 1], ig5[:, :, 4], ALU.add, R=[r_ig5, r_ig[e_]], W=[r_ig[e_]])
            cp("dve", idx_i[:, e_, :], ig[:, e_, :, 0], R=[r_ig[e_]], W=[r_idx[e_]])
            yield

        def gather(e_):
            for s in range(4):
                P.dma("pool", lambda e, s=s: e.indirect_dma_start(out=xe[:, s, :], out_offset=None, in_=u2_d[:, :],
                                                                  in_offset=bass.IndirectOffsetOnAxis(ap=idx_i[:, e_, s:s + 1], axis=0)),
                      [r_idx[e_], r_u2d], [r_xe])

        ptb_m = banks[2].bitcast(BF).rearrange("p (a b) -> p a b", b=128)

        def xpose(s):
            for dk in range(8):
                tr(ptb_m[:, dk, :], xe[:, s, dk * 128:(dk + 1) * 128], ident_bf[:, :], R=[r_xe, r_const], W=[bR[2]])
            cp("dve" if s % 2 == 0 else "act", xeT[:, :, s * 128:(s + 1) * 128], ptb_m[:, :, :], R=[bR[2]], W=[r_xeT])

        def expert(e_, bg):
            def tick():
                if bg is not None:
                    next(bg, None)
            wi = e_ * 3
            Wg = wring[wi % 4]; Wu = wring[(wi + 1) % 4]; Wd = wring[(wi + 2) % 4]
            rWg = r_wring[wi % 4]; rWu = r_wring[(wi + 1) % 4]; rWd = r_wring[(wi + 2) % 4]
            for fc in range(8):
                pG = banks[3 + fc % 2]; pU = banks[5 + fc % 2]
                for dk in range(8):
                    mm(pG[:, :], Wg[:, dk, fc * 128:(fc + 1) * 128], xeT[:, dk, :], start=(dk == 0), stop=(dk == 7), R=[rWg, r_xeT], W=[bR[3 + fc % 2]])
                for dk in range(8):
                    mm(pU[:, :], Wu[:, dk, fc * 128:(fc + 1) * 128], xeT[:, dk, :], start=(dk == 0), stop=(dk == 7), R=[rWu, r_xeT], W=[bR[5 + fc % 2]])
                act(sg[fc % 2][:, :], pG[:, :], AF.Silu, R=[bR[3 + fc % 2]], W=[r_sg[fc % 2]])
                tt("dve", hT[:, fc, :], pU[:, :], sg[fc % 2][:, :], ALU.mult, R=[bR[5 + fc % 2], r_sg[fc % 2]], W=[r_hT])
                tick()
            load_next_w(); load_next_w()
            for s in range(4):
                y = ye[s]
                for hf in range(2):
                    pY = banks[7] if hf == 0 else banks[1]
                    rY = bR[7] if hf == 0 else bR[1]
                    for fc in range(8):
                        mm(pY[:, :], hT[:, fc, s * 128:(s + 1) * 128], Wd[:, fc, hf * 512:(hf + 1) * 512], start=(fc == 0), stop=(fc == 7),
                           R=[r_hT, rWd], W=[rY])
                    stt("dve", y[:, hf * 512:(hf + 1) * 512], pY[:, :], ig[:, e_, s, 1:2], g2_bc[:, hf * 512:(hf + 1) * 512], ALU.mult, ALU.mult,
                        R=[rY, r_ig[e_], r_bc2], W=[r_ye[s]])
                    tick()
                if e_ + 1 < NE:
                    xpose(s)
                P.dma("pool", lambda e, s=s, y=y: e.indirect_dma_start(out=acc_d[:, :], out_offset=bass.IndirectOffsetOnAxis(ap=idx_i[:, e_, s:s + 1], axis=0),
                                                                       in_=y[:, :], in_offset=None, compute_op=ALU.add),
                      [r_idx[e_], r_ye[s], r_accd] + (r_acc_e[e_ - 1] if e_ > 0 else []), [r_acc_e[e_][s]])
            load_next_w()

        for _ in compact(0):
            pass
        for _ in compact(1):
            pass
        gather(0)
        for s in range(4):
            xpose(s)
        gather(1)
        for e_ in range(NE):
            bg = compact(e_ + 2) if e_ + 2 < NE else None
            expert(e_, bg)
            if bg is not None:
                for _ in bg:
                    pass
            if e_ + 2 < NE:
                gather(e_ + 2)

        P.barrier()
        cur[0] = MOE_BASE
        fx = [alloc([128, D]) for _ in range(4)]; r_fx = [Res() for _ in range(4)]
        fo = [alloc([128, D]) for _ in range(4)]; r_fo = [Res() for _ in range(4)]
        r_out = [Res() for _ in range(32)]

        def fchain(T):
            i = T % 4
            dma("sp", fx[i][:, :], acc_d[T * 128:(T + 1) * 128, :], R=[r_accd, r_acc_e[NE - 1]], W=[r_fx[i]])
            yield
            yield from ln_stats_g(fx[i], r_fx[i], i, eps=LN_EPS / (ALPHA * ALPHA))
            stt("dve", fo[i][:, :], fx[i][:, :], mv[i][:, 0:1], ln2g_bc[:, :], ALU.subtract, ALU.mult, R=[r_fx[i], r_ln[i], r_bc2], W=[r_fo[i]])
            stt("dve", fo[i][:, :], fo[i][:, :], rstd[i][:, 0:1], ln2b_bc[:, :], ALU.mult, ALU.add, R=[r_fo[i], r_ln[i], r_bc2], W=[r_fo[i]])
            yield
            dma("sp", out_d[T * 128:(T + 1) * 128, :], fo[i][:, :], R=[r_fo[i]], W=[r_out[T]])
        for T0 in range(0, 32, 4):
            run_il([fchain(T) for T in range(T0, T0 + 4)])
        P.barrier()
        P.emit(block)
    return nc


def _na_tables(rel_bias):
    rb = np.asarray(rel_bias, np.float32)
    qrs = [4, 5, 0, 1, 2, 3, 60, 61, 62, 63]
    out = np.full((10, NH, 5, 64, 128), NEG, np.float32)
    qc = np.arange(64)
    c0 = np.clip(qc - 8, 0, 48)
    for v, qr in enumerate(qrs):
        r0 = min(max(qr - 4, 0), 56)
        kt0 = r0 // 2
        nt = 4 if r0 % 2 == 0 else 5
        for ci in range(nt):
            for w2 in range(2):
                kr = 2 * (kt0 + ci) + w2
                if not (r0 <= kr < r0 + 8):
                    continue
                dr = kr - qr + 7
                for kc in range(64):
                    ok = (c0 <= kc) & (kc < c0 + 16)
                    dc = kc - qc + 15
                    vals = rb[:, dr, np.clip(dc, 0, 30)]
                    out[v, :, ci, :, w2 * 64 + kc] = np.where(ok[None, :], vals, NEG)
    return out


def _rope_tables():
    t = np.arange(SEQ)
    row = (t // 64).astype(np.float32)
    col = (t % 64).astype(np.float32)
    inv = (10000.0 ** (-np.arange(0, 16, 2, dtype=np.float32) / 16)).astype(np.float32)
    ang = np.concatenate([row[:, None] * inv, col[:, None] * inv], axis=-1).astype(np.float32)
    cos = np.cos(ang).astype(np.float32).T
    sin = np.sin(ang).astype(np.float32).T
    C = np.concatenate([np.ones((64, SEQ), np.float32), cos, cos], 0)
    S = np.concatenate([np.zeros((64, SEQ), np.float32), -sin, sin], 0)
    return np.ascontiguousarray(C), np.ascontiguousarray(S)


_CACHE = {}


def kernel(x, c, ctx, c_ctx, w_mod, b_mod, w_in, q_norm_g, w_uq, kv_norm_g, w_ukv, na_rel_bias,
           w_proj_mla, w_proj_na, w_out, ln1_g, ln1_b, w_router, w_exp_gate, w_exp_up, w_exp_down,
           ln2_g, ln2_b, _debug=False):
    f = lambda a: np.ascontiguousarray(np.asarray(a, dtype=np.float32))
    x = f(x); c = f(c); ctx = f(ctx); c_ctx = f(c_ctx)
    key = "dbg" if _debug else "nc"
    if key not in _CACHE:
        _CACHE[key] = build_program(debug=_debug)
    nc = _CACHE[key]
    C, S = _rope_tables()
    _nt = _na_tables(f(na_rel_bias)[0])
    _NT = (np.ascontiguousarray(np.concatenate([_nt[0, :, 0:4], _nt[1, :, 0:5]], axis=1).transpose(2, 0, 1, 3)),
           np.ascontiguousarray(_nt[2:10, :, 0:4].transpose(0, 3, 1, 2, 4)))
    shared = {
        "w_mod": f(w_mod)[0],
        "bmod_fm": np.ascontiguousarray(f(b_mod)[0].reshape(48, 128).T),
        "bmod_row": f(b_mod)[0].reshape(1, 6 * D),
        "w_in": f(w_in)[0],
        "qg": np.ascontiguousarray(f(q_norm_g)[0].reshape(2, 128).T),
        "kvg": f(kv_norm_g)[0].reshape(128, 1),
        "w_uq": f(w_uq)[0],
        "w_ukv": f(w_ukv)[0],
        "natab01": _NT[0],
        "natabe": _NT[1],
        "w_proj_mla": f(w_proj_mla)[0],
        "w_proj_na": f(w_proj_na)[0],
        "w_out": f(w_out)[0],
        "lnv": np.ascontiguousarray(np.stack([f(ln1_g)[0], f(ln1_b)[0], f(ln2_g)[0], f(ln2_b)[0]], 0)),
        "w_router": f(w_router)[0],
        "w_exp_gate": f(w_exp_gate)[0],
        "w_exp_up": f(w_exp_up)[0],
        "w_exp_down": f(w_exp_down)[0],
        "ropec": C,
        "ropes": S,
    }
    in_maps = []
    for b in range(8):
        cc = np.stack([c[b].reshape(8, 128).T, c_ctx.reshape(8, 128).T], axis=-1)
        m = dict(shared)
        m["x"] = x[b]
        m["ctx"] = ctx[b]
        m["cc"] = np.ascontiguousarray(cc)
        in_maps.append(m)
    res = run_bass_kernel_spmd(nc, in_maps, core_ids=list(range(8)))
    out = np.stack([np.asarray(r["out"], dtype=np.float32) for r in res.results], axis=0)
    if _debug:
        return out, res.results
    return out
```
